# Optimizing a Trainium2 kernel written in Bass

```python
import math
import jax, jax.numpy as jnp
from jax import lax
import numpy as np

D_MODEL = 1024
BATCH = 8
SEQ = 4096
DEPTH = 2

BRANCH_WIDTH = 512
N_BRANCHES = 4
RET_HEADS = 4
RET_QK_DIM = 64
RET_V_DIM = 128
RET_CHUNK = 128
SWA_Q_HEADS = 8
SWA_KV_HEADS = 2
SWA_HEAD_DIM = 64
SWA_WINDOW = 128
SWA_BLOCK = 128
POOL_WINDOWS = (2, 4, 8, 16)
POOL_GROUP_DIM = BRANCH_WIDTH // 4
M_HEAD_DIM = 64
M_HEADS = BRANCH_WIDTH // M_HEAD_DIM
M_GROUPS = 2
M_STATE = 128
M_CONV = 4
M_CHUNK = 128
M_BC_W = M_GROUPS * M_STATE
M_CONV_DIM = BRANCH_WIDTH + 2 * M_BC_W
EPS = 1e-6

RET_QK_W = RET_HEADS * RET_QK_DIM
RET_V_W = RET_HEADS * RET_V_DIM
SWA_Q_W = SWA_Q_HEADS * SWA_HEAD_DIM
SWA_KV_W = SWA_KV_HEADS * SWA_HEAD_DIM
IN_SPLIT_SIZES = (RET_QK_W, RET_QK_W, RET_V_W, BRANCH_WIDTH,
                  SWA_Q_W, SWA_KV_W, SWA_KV_W, BRANCH_WIDTH,
                  BRANCH_WIDTH, BRANCH_WIDTH,
                  M_CONV_DIM, BRANCH_WIDTH, M_HEADS,
                  N_BRANCHES * D_MODEL)
D_IN = sum(IN_SPLIT_SIZES)

kernel_name = 'hybrid_gated_retention_swa_pool_ssd'


def _rms(x):
    xf = x.astype(jnp.float32)
    return xf * lax.rsqrt(jnp.mean(xf * xf, axis=-1, keepdims=True) + EPS)


def rms_norm(x, w):
    return (_rms(x) * w.astype(jnp.float32)).astype(x.dtype)


def alibi_slopes(n):
    return 2.0 ** (-8.0 * jnp.arange(1, n + 1, dtype=jnp.float32) / n)


def retention(q, k, v):
    b, s, h, dk = q.shape
    dv = v.shape[-1]
    L = RET_CHUNK
    nc = s // L
    log_g = jnp.log(1.0 - 2.0 ** (-5.0 - jnp.arange(h, dtype=jnp.float32)))
    pos = jnp.arange(L, dtype=jnp.float32)
    diff = pos[:, None] - pos[None, :]
    causal = diff >= 0
    inner_decay = jnp.where(causal, jnp.exp(log_g[:, None, None] * jnp.where(causal, diff, 0.0)), 0.0)
    q_decay = jnp.exp(log_g[:, None] * (pos + 1.0))
    k_decay = jnp.exp(log_g[:, None] * (L - 1.0 - pos))
    chunk_decay = jnp.exp(log_g * L)
    qc = q.reshape(b, nc, L, h, dk)
    kc = (k * dk ** -0.5).reshape(b, nc, L, h, dk)
    vc = v.reshape(b, nc, L, h, dv)
    scores = jnp.einsum('bclhd,bcshd->bchls', qc, kc) * inner_decay
    inner = jnp.einsum('bchls,bcshe->bclhe', scores, vc)

    def step(state, inp):
        q_i, k_i, v_i = inp
        cross = jnp.einsum('blhd,bhde,hl->blhe', q_i, state, q_decay)
        new = state * chunk_decay[None, :, None, None] + jnp.einsum('blhd,hl,blhe->bhde', k_i, k_decay, v_i)
        return new, cross

    init = jnp.zeros((b, h, dk, dv), jnp.float32)
    _, cross = lax.scan(step, init, (jnp.moveaxis(qc, 1, 0), jnp.moveaxis(kc, 1, 0), jnp.moveaxis(vc, 1, 0)))
    out = inner + jnp.moveaxis(cross, 0, 1)
    return out.reshape(b, s, h, dv)


def sliding_window_attention(q, k, v, sinks):
    b, s, hq, d = q.shape
    hkv = k.shape[2]
    g = hq // hkv
    L = SWA_BLOCK
    nb = s // L
    qb = (q * d ** -0.5).reshape(b, nb, L, hkv, g, d)

    def with_prev(t):
        tb = t.reshape(b, nb, L, hkv, d)
        prev = jnp.concatenate([jnp.zeros_like(tb[:, :1]), tb[:, :-1]], axis=1)
        return jnp.concatenate([prev, tb], axis=2)

    kb = with_prev(k)
    vb = with_prev(v)
    scores = jnp.einsum('bnqhgd,bnkhd->bnhgqk', qb, kb).astype(jnp.float32)
    qi = jnp.arange(L)
    kj = jnp.arange(2 * L)
    delta = L + qi[:, None] - kj[None, :]
    key_pos = (jnp.arange(nb)[:, None] - 1) * L + kj[None, :]
    valid = ((delta >= 0) & (delta < SWA_WINDOW))[None] & (key_pos >= 0)[:, None, :]
    slopes = alibi_slopes(hq).reshape(hkv, g)
    scores = scores - slopes[:, :, None, None] * delta.astype(jnp.float32)
    scores = jnp.where(valid[None, :, None, None], scores, -jnp.inf)
    sink = sinks.astype(jnp.float32).reshape(hkv, g)[None, None, :, :, None, None]
    m = jnp.maximum(scores.max(axis=-1, keepdims=True), sink)
    p = jnp.exp(scores - m)
    probs = p / (p.sum(axis=-1, keepdims=True) + jnp.exp(sink - m))
    out = jnp.einsum('bnhgqk,bnkhd->bnqhgd', probs.astype(v.dtype), vb)
    return out.reshape(b, s, hq * d)


def multiscale_pool(u, w_grp, scale):
    b, s, cdim = u.shape
    gd = cdim // len(POOL_WINDOWS)
    uf = u.astype(jnp.float32)
    cs = jnp.cumsum(uf, axis=1)
    t = jnp.arange(s)
    outs = []
    for i, w in enumerate(POOL_WINDOWS):
        cs_g = cs[..., i * gd:(i + 1) * gd]
        lag = jnp.pad(cs_g, ((0, 0), (w, 0), (0, 0)))[:, :s]
        count = jnp.minimum(t + 1, w).astype(jnp.float32)[None, :, None]
        outs.append((cs_g - lag) / count - uf[..., i * gd:(i + 1) * gd])
    diff = jnp.stack(outs, axis=2)
    y = jnp.einsum('bsgc,gce->bsge', diff, w_grp).reshape(b, s, cdim)
    return y * scale


def causal_depthwise_conv(u, w, bias):
    k, ch = w.shape
    out = lax.conv_general_dilated(u, w[:, None, :].astype(u.dtype), window_strides=(1,),
                                   padding=((k - 1, 0),), dimension_numbers=('NWC', 'WIO', 'NWC'),
                                   feature_group_count=ch)
    return out + bias


def ssd_scan(x, dt, a, bmat, cmat, d_skip):
    b, s, h, p = x.shape
    grp, n = bmat.shape[2], bmat.shape[3]
    r = h // grp
    L = M_CHUNK
    nc = s // L
    xc = x.reshape(b, nc, L, grp, r, p)
    dtc = dt.reshape(b, nc, L, grp, r)
    acs = jnp.cumsum(dtc * a.reshape(grp, r), axis=2)
    bc = bmat.reshape(b, nc, L, grp, n)
    cc = cmat.reshape(b, nc, L, grp, n)
    xdt = xc * dtc[..., None]
    acs_t = jnp.moveaxis(acs, 2, -1)
    causal = jnp.tril(jnp.ones((L, L), dtype=bool))
    seg = acs_t[..., :, None] - acs_t[..., None, :]
    lmat = jnp.exp(jnp.where(causal, seg, -jnp.inf))
    cb = jnp.einsum('bclgn,bcsgn->bcgls', cc, bc)
    y_diag = jnp.einsum('bcgls,bcgrls,bcsgrp->bclgrp', cb, lmat, xdt)
    decay_to_end = jnp.exp(acs[:, :, -1:] - acs)

    def step(state, inp):
        b_i, c_i, acs_i, dend_i, xdt_i = inp
        y_off = jnp.einsum('blgn,bgrpn,blgr->blgrp', c_i, state, jnp.exp(acs_i))
        new = state * jnp.exp(acs_i[:, -1])[..., None, None] + jnp.einsum('blgn,blgr,blgrp->bgrpn', b_i, dend_i, xdt_i)
        return new, y_off

    init = jnp.zeros((b, grp, r, p, n), jnp.float32)
    xs = (jnp.moveaxis(bc, 1, 0), jnp.moveaxis(cc, 1, 0), jnp.moveaxis(acs, 1, 0),
          jnp.moveaxis(decay_to_end, 1, 0), jnp.moveaxis(xdt, 1, 0))
    _, y_off = lax.scan(step, init, xs)
    y = y_diag + jnp.moveaxis(y_off, 0, 1) + xc * d_skip.reshape(grp, r)[:, :, None]
    return y.reshape(b, s, h * p)


def hybrid_layer(x, c, ada_w, ada_b, norm_w, w_in, sinks, pool_w, pool_scale,
                 conv_w, conv_b, dt_bias, a_log, d_skip, ssm_norm_w, w_up, w_out):
    b, s, _ = x.shape
    mod = jnp.einsum('bd,de->be', jax.nn.silu(c), ada_w) + ada_b
    shift, scale, gate = jnp.split(mod, 3, axis=-1)
    h = rms_norm(x, norm_w) * (1.0 + scale[:, None, :]) + shift[:, None, :]
    proj = jnp.einsum('bsd,de->bse', h, w_in)
    offsets = np.cumsum(IN_SPLIT_SIZES)[:-1].tolist()
    (rq, rk, rv, rg, sq, sk, sv, sg, px, pg, mxbc, mz, mdt, mg) = jnp.split(proj, offsets, axis=-1)

    ret = retention(rq.reshape(b, s, RET_HEADS, RET_QK_DIM), rk.reshape(b, s, RET_HEADS, RET_QK_DIM),
                    rv.reshape(b, s, RET_HEADS, RET_V_DIM))
    ret = _rms(ret).reshape(b, s, BRANCH_WIDTH) * jax.nn.silu(rg.astype(jnp.float32))

    att = sliding_window_attention(sq.reshape(b, s, SWA_Q_HEADS, SWA_HEAD_DIM),
                                   sk.reshape(b, s, SWA_KV_HEADS, SWA_HEAD_DIM),
                                   sv.reshape(b, s, SWA_KV_HEADS, SWA_HEAD_DIM), sinks)
    att = att * jax.nn.silu(sg)

    pool = multiscale_pool(px, pool_w, pool_scale) * jax.nn.silu(pg.astype(jnp.float32))

    xbc = jax.nn.silu(causal_depthwise_conv(mxbc, conv_w, conv_b))
    mx, mb, mc = jnp.split(xbc, [BRANCH_WIDTH, BRANCH_WIDTH + M_BC_W], axis=-1)
    dt = jax.nn.softplus(mdt.astype(jnp.float32) + dt_bias.astype(jnp.float32))
    a = -jnp.exp(a_log.astype(jnp.float32))
    y = ssd_scan(mx.reshape(b, s, M_HEADS, M_HEAD_DIM), dt, a,
                 mb.reshape(b, s, M_GROUPS, M_STATE), mc.reshape(b, s, M_GROUPS, M_STATE), d_skip)
    yz = (y * jax.nn.silu(mz.astype(jnp.float32))).reshape(b, s, M_GROUPS, BRANCH_WIDTH // M_GROUPS)
    ssm = _rms(yz).reshape(b, s, BRANCH_WIDTH) * ssm_norm_w.astype(jnp.float32)

    branches = jnp.stack([ret.astype(x.dtype), att.astype(x.dtype), pool.astype(x.dtype), ssm.astype(x.dtype)], axis=2)
    up = jnp.einsum('bsne,ned->bsnd', branches, w_up)
    gates = jax.nn.sigmoid(mg.astype(jnp.float32)).reshape(b, s, N_BRANCHES, D_MODEL)
    merged = jnp.sum(gates * up, axis=2).astype(x.dtype)
    out = jnp.einsum('bsd,de->bse', merged, w_out)
    return x + gate[:, None, :] * out


def setup_inputs(seed: int = 0) -> dict:
    key = jax.random.key(seed)
    ks = jax.random.split(key, 20)
    f32 = jnp.float32
    gd = POOL_GROUP_DIM
    dt_u = jax.random.uniform(ks[11], (DEPTH, M_HEADS), f32)
    dt0 = jnp.exp(dt_u * (math.log(0.1) - math.log(0.001)) + math.log(0.001))
    return {
        'x': jax.random.normal(ks[0], (BATCH, SEQ, D_MODEL), f32),
        'c': jax.random.normal(ks[1], (BATCH, D_MODEL), f32),
        'ada_w': jax.random.normal(ks[2], (DEPTH, D_MODEL, 3 * D_MODEL), f32) * D_MODEL ** -0.5,
        'ada_b': jax.random.normal(ks[3], (DEPTH, 3 * D_MODEL), f32) * 0.02,
        'norm_w': 1.0 + 0.02 * jax.random.normal(ks[4], (DEPTH, D_MODEL), f32),
        'w_in': jax.random.normal(ks[5], (DEPTH, D_MODEL, D_IN), f32) * D_MODEL ** -0.5,
        'swa_sinks': jax.random.normal(ks[6], (DEPTH, SWA_Q_HEADS), f32),
        'pool_w': jax.random.normal(ks[7], (DEPTH, len(POOL_WINDOWS), gd, gd), f32) * gd ** -0.5,
        'pool_scale': 1.0 + 0.02 * jax.random.normal(ks[8], (DEPTH, BRANCH_WIDTH), f32),
        'conv_w': jax.random.normal(ks[9], (DEPTH, M_CONV, M_CONV_DIM), f32) * M_CONV ** -0.5,
        'conv_b': jax.random.normal(ks[10], (DEPTH, M_CONV_DIM), f32) * 0.02,
        'dt_bias': dt0 + jnp.log(-jnp.expm1(-dt0)),
        'a_log': jnp.log(jax.random.uniform(ks[12], (DEPTH, M_HEADS), f32, minval=1.0, maxval=16.0)),
        'd_skip': 1.0 + 0.02 * jax.random.normal(ks[13], (DEPTH, M_HEADS), f32),
        'ssm_norm_w': 1.0 + 0.02 * jax.random.normal(ks[14], (DEPTH, BRANCH_WIDTH), f32),
        'w_up': jax.random.normal(ks[15], (DEPTH, N_BRANCHES, BRANCH_WIDTH, D_MODEL), f32) * BRANCH_WIDTH ** -0.5,
        'w_out': jax.random.normal(ks[16], (DEPTH, D_MODEL, D_MODEL), f32) * D_MODEL ** -0.5,
        'final_norm_w': 1.0 + 0.02 * jax.random.normal(ks[17], (D_MODEL,), f32),
    }


def reference(x, c, ada_w, ada_b, norm_w, w_in, swa_sinks, pool_w, pool_scale, conv_w, conv_b,
              dt_bias, a_log, d_skip, ssm_norm_w, w_up, w_out, final_norm_w):
    for l in range(DEPTH):
        x = hybrid_layer(x, c, ada_w[l], ada_b[l], norm_w[l], w_in[l], swa_sinks[l], pool_w[l],
                         pool_scale[l], conv_w[l], conv_b[l], dt_bias[l], a_log[l], d_skip[l],
                         ssm_norm_w[l], w_up[l], w_out[l])
    return rms_norm(x, final_norm_w)
```

```python
import numpy as np
from contextlib import ExitStack
import concourse.bass as bass
import concourse.mybir as mybir
from concourse.bass_utils import run_bass_kernel_spmd

F32 = mybir.dt.float32
BF16 = mybir.dt.bfloat16
AF = mybir.ActivationFunctionType
ALU = mybir.AluOpType

D = 1024
T = 512
NCH = 4
KC = 8
NV = NCH + 1
EPS = 1e-6
NSLOT = 3
SLOTW = 4096
ENGS = ("pe", "act", "dve", "pool", "sp")

O_RQ, O_RK, O_RV, O_RG = 0, 256, 512, 1024
O_SQ, O_SK, O_SV, O_SG = 1536, 2048, 2176, 2304
O_PX, O_PG = 2816, 3328
O_XBC, O_MZ, O_MDT, O_MG = 3840, 4864, 5376, 5384


class Sched:
    def __init__(self):
        self.ops = {e: [] for e in ENGS}
        self.cnt = {}
        self.waited = {e: {} for e in ENGS}
        self.res = {}
        self.sem_keys = []
        self.finals = []
        self.phase = "setup"
        self.pe_tags = []

    def _sem(self, k):
        if k not in self.cnt:
            self.cnt[k] = 0
            self.sem_keys.append(k)

    def op(self, eng, fn, reads=(), writes=(), dma=None):
        need = {}

        def want(tok):
            if tok is None:
                return
            s, v = tok
            if s == "c_" + eng and eng == "pe":
                return
            if need.get(s, 0) < v:
                need[s] = v

        for r in reads:
            st = self.res.get(r)
            if st is not None:
                want(st[0])
                if r.startswith("ps") or r.startswith("pt"):
                    for s_, v_ in st[1].items():
                        want((s_, v_))
        for w in writes:
            st = self.res.get(w)
            if st is not None:
                want(st[0])
                for s, v in st[1].items():
                    want((s, v))
        waits = []
        wd = self.waited[eng]
        for s, v in need.items():
            if wd.get(s, 0) < v:
                wd[s] = v
                waits.append((s, v))
        if dma is not None:
            k, inc = dma, 16
        else:
            k, inc = "c_" + eng, 1
        self._sem(k)
        self.cnt[k] += inc
        tok = (k, self.cnt[k])
        if eng == "pe":
            self.pe_tags.append((self.phase, getattr(fn, "nins", 1)))
        self.ops[eng].append((waits, fn, k, inc))
        for r in reads:
            st = self.res.setdefault(r, [None, {}])
            if st[1].get(k, 0) < tok[1]:
                st[1][k] = tok[1]
        for w in writes:
            self.res[w] = [tok, {}]
        return tok

    def emit(self, nc):
        with ExitStack() as st:
            sems = {k: st.enter_context(nc.semaphore(k)) for k in self.sem_keys}
            block = st.enter_context(nc.Block())
            finals = {}
            for eng, toks in self.finals:
                finals.setdefault(eng, []).extend(toks)

            def mk(ename):
                def body(e):
                    for waits, fn, k, inc in self.ops[ename]:
                        for s, v in waits:
                            e.wait_ge(sems[s], v)
                        fn(e).then_inc(sems[k], inc)
                    for s, v in finals.get(ename, []):
                        e.wait_ge(sems[s], v)
                return body

            block.sync(mk("sp"))
            block.tensor(mk("pe"))
            block.scalar(mk("act"))
            block.vector(mk("dve"))
            block.gpsimd(mk("pool"))


def _gran_cols():
    ar = np.arange
    perm_att = np.concatenate([np.concatenate([jj * 64 + ar(64), (4 + jj) * 64 + ar(64)]) for jj in range(4)])
    g = {}
    g["A_v"] = O_RV + ar(512)
    g["A_qk"] = np.concatenate([O_RQ + ar(256), O_RK + ar(256)])
    g["A_g"] = O_RG + ar(512)
    g["A_k"] = np.concatenate([O_RK + ar(256), O_MDT + ar(8)])
    g["B_q"] = O_SQ + perm_att
    g["B_kv"] = np.concatenate([O_SK + ar(128), O_SV + np.concatenate([ar(64), ar(64), 64 + ar(64), 64 + ar(64)])])
    g["B_g"] = O_SG + perm_att
    g["C_u"] = O_PX + ar(512)
    g["C_g"] = O_PG + ar(512)
    g["D_x0"] = O_XBC + ar(512)
    g["D_x1"] = O_XBC + 512 + ar(512)
    g["D_z"] = O_MZ + ar(512)
    for i in range(4):
        g["mg%d_0" % i] = O_MG + i * 1024 + ar(512)
        g["mg%d_1" % i] = O_MG + i * 1024 + 512 + ar(512)
    return g, perm_att


STREAM = []
for _i, (_m, _gs) in enumerate([("A", ["A_v", "A_qk", "A_g", "A_k"]), ("B", ["B_q", "B_kv", "B_g"]),
                                ("C", ["C_u", "C_g"]), ("D", ["D_x0", "D_x1", "D_z"])]):
    STREAM += _gs + ["mg%d_0" % _i, "up%d_0" % _i, "mg%d_1" % _i, "up%d_1" % _i]
STREAM += ["wo_0", "wo_1"]
ADA = ["ada%d" % i for i in range(6)]


def _kc_layout(w):
    n = w.shape[1]
    return np.ascontiguousarray(w.reshape(KC, 128, n).transpose(1, 0, 2)).reshape(128, KC * n)


def host_weights(inp, l):
    gcols, perm_att = _gran_cols()
    W = inp["w_in"][l]
    parts, offs, o = [], {}, 0
    for name in ADA + STREAM:
        if name.startswith("ada"):
            i = int(name[3:])
            a = _kc_layout(inp["ada_w"][l][:, i * 512:(i + 1) * 512])
        elif name.startswith("up"):
            i = int(name[2]); hf = int(name[4])
            wu = inp["w_up"][l][i]
            if i == 1:
                wu = wu[perm_att]
            wu = wu[:, hf * 512:(hf + 1) * 512]
            a = np.ascontiguousarray(wu.reshape(4, 128, 512).transpose(1, 0, 2)).reshape(128, 2048)
        elif name.startswith("wo"):
            i = int(name[3:])
            a = _kc_layout(inp["w_out"][l][:, i * 512:(i + 1) * 512])
        else:
            a = _kc_layout(W[:, gcols[name]])
        offs[name] = (o, a.shape[1])
        o += a.shape[1]
        parts.append(a)
    return np.ascontiguousarray(np.concatenate(parts, axis=1), dtype=np.float32), offs


def _weight_offsets():
    gcols, _ = _gran_cols()
    offs, o = {}, 0
    for name in ADA + STREAM:
        if name.startswith("ada") or name.startswith("wo"):
            n = 4096
        elif name.startswith("up"):
            n = 2048
        else:
            n = KC * len(gcols[name])
        offs[name] = (o, n)
        o += n
    return offs, o


CF = {}
CB = {}


def make_consts():
    p = np.arange(128)
    s = np.arange(128)[:, None].astype(np.float64)
    l = np.arange(128)[None, :].astype(np.float64)
    gam = 1.0 - 2.0 ** (-5.0 - np.arange(4))
    rowmask = np.stack([(p < 64), (p >= 64)], 1).astype(np.float64)
    f = {}
    f["rowmask8"] = rowmask / 8.0
    f["qdm"] = np.stack([rowmask[:, h % 2][:, None] * gam[h] ** (np.arange(128) + 1.0)[None, :] for h in range(4)], 1).reshape(128, 512)
    f["maskTA"] = np.stack([(s <= l) * gam[h] ** (-(s + 1.0)) / 8.0 for h in range(4)], 1).reshape(128, 512)
    f["kdec"] = np.stack([gam[h] ** (127.0 - np.arange(128)) / 8.0 for h in range(4)], 1)
    f["gL"] = np.stack([np.where(p < 64, gam[2 * j] ** 128, gam[2 * j + 1] ** 128) for j in range(2)], 1)
    f["tri"] = (s <= l).astype(np.float64)
    sel = np.zeros((128, 12, 128))
    for h in range(8):
        sel[h, h, :] = 1.0
    for j in range(4):
        sel[2 * j, 8 + j, :64] = 1.0
        sel[2 * j + 1, 8 + j, 64:] = 1.0
    f["sel"] = sel.reshape(128, 12 * 128)
    f["ones"] = np.ones((128, 128))
    f["epsc"] = np.tile(np.array([EPS, EPS, EPS])[None, :], (128, 1))
    b = {}
    slopes = 2.0 ** (-(np.arange(8) + 1.0))
    k = np.arange(128)[:, None].astype(np.float64)
    q = np.arange(128)[None, :].astype(np.float64)
    dec = np.zeros((128, 2, 2, 4, 128))
    for g in range(2):
        for jj in range(4):
            sl = slopes[4 * g + jj]
            dec[:, g, 0, jj, :] = np.where(k > q, np.exp(-sl * (128.0 + q - k)), 0.0)
            dec[:, g, 1, jj, :] = np.where(k <= q, np.exp(-sl * (q - k)), 0.0)
    b["swadec"] = dec.reshape(128, 2048)
    M = np.zeros((128, 3, 4, 128))
    for i, w in enumerate((2, 4, 8, 16)):
        dlt = q - k
        M[:, 0, i, :] = (1.0 / w) * ((dlt >= 0) & (dlt <= w - 1)) - (dlt == 0)
        M[:, 1, i, :] = (1.0 / w) * ((q - (k - 128.0)) <= w - 1)
        cnt = np.minimum(q + 1.0, w)
        M[:, 2, i, :] = (1.0 / cnt) * ((dlt >= 0) & (dlt <= w - 1)) - (dlt == 0)
    b["poolM"] = M.reshape(128, 12 * 128)
    b["onesb"] = np.ones((128, 128))
    b["ident"] = np.eye(128)
    fo, o = {}, 0
    for kname, v in f.items():
        fo[kname] = (o, v.shape[1]); o += v.shape[1]
    bo, ob = {}, 0
    for kname, v in b.items():
        bo[kname] = (ob, v.shape[1]); ob += v.shape[1]
    cf = np.concatenate([f[kname] for kname in f], 1).astype(np.float32)
    cb = np.concatenate([b[kname] for kname in b], 1).astype(np.float32)
    return cf, fo, cb, bo


PCOL = {"nw": (0, 8), "adab": (8, 24), "pscale": (32, 4), "convw": (36, 32), "convb": (68, 8),
        "dskip": (76, 4), "ssmw": (80, 4), "fnw": (84, 8)}
NPCOL = 92
PROW = {"dtb": (0, 32), "alog": (32, 32), "sinks": (64, 8)}
NPROW = 72


def host_small(inp, l):
    pc = np.zeros((128, NPCOL), np.float32)
    pc[:, 0:8] = inp["norm_w"][l].reshape(8, 128).T
    pc[:, 8:32] = inp["ada_b"][l].reshape(24, 128).T
    pc[:, 32:36] = inp["pool_scale"][l].reshape(4, 128).T
    cw = inp["conv_w"][l]
    pc[:, 36:68] = cw.reshape(4, 8, 128).transpose(2, 1, 0).reshape(128, 32)
    pc[:, 68:76] = inp["conv_b"][l].reshape(8, 128).T
    ds = inp["d_skip"][l]
    pc[:, 76:80] = np.stack([np.where(np.arange(128) < 64, ds[2 * j], ds[2 * j + 1]) for j in range(4)], 1)
    pc[:, 80:84] = inp["ssm_norm_w"][l].reshape(4, 128).T
    pc[:, 84:92] = inp["final_norm_w"].reshape(8, 128).T
    pr = np.zeros((128, NPROW), np.float32)
    pr[:, 0:32] = np.tile(inp["dt_bias"][l], NCH)[None, :]
    pr[:, 32:64] = np.tile(inp["a_log"][l], NCH)[None, :]
    pr[:, 64:72] = inp["swa_sinks"][l][None, :]
    pw = np.ascontiguousarray(inp["pool_w"][l].transpose(1, 0, 2)).reshape(128, 512)
    return pc, pr, pw.astype(np.float32)


def build_program(NTOK, DEPTH):
    NT = NTOK // T
    woffs, WTOT = _weight_offsets()
    cf_np, FO, cb_np, BO = make_consts()
    NCF, NCB = cf_np.shape[1], cb_np.shape[1]
    nc = bass.Bass("TRN2", target_bir_lowering=False)
    xT_d = nc.dram_tensor("xT", [D, NTOK], F32, kind="ExternalInput").ap()
    outT_d = nc.dram_tensor("outT", [D, NTOK], F32, kind="ExternalOutput").ap()
    ccol_d = nc.dram_tensor("ccol", [128, 8], F32, kind="ExternalInput").ap()
    cstf_d = nc.dram_tensor("cstf", [128, NCF], F32, kind="ExternalInput").ap()
    cstb_d = nc.dram_tensor("cstb", [128, NCB], F32, kind="ExternalInput").ap()
    wts_d = [nc.dram_tensor("wts%d" % l, [128, WTOT], F32, kind="ExternalInput").ap() for l in range(DEPTH)]
    pcol_d = [nc.dram_tensor("pcol%d" % l, [128, NPCOL], F32, kind="ExternalInput").ap() for l in range(DEPTH)]
    prow_d = [nc.dram_tensor("prow%d" % l, [128, NPROW], F32, kind="ExternalInput").ap() for l in range(DEPTH)]
    poolw_d = [nc.dram_tensor("poolw%d" % l, [128, 512], F32, kind="ExternalInput").ap() for l in range(DEPTH)]
    xmid_d = [nc.dram_tensor("xmid%d" % l, [D, NTOK], F32).ap() for l in range(DEPTH - 1)]
    wbf_d = [nc.dram_tensor("wbf%d" % l, [128, WTOT], BF16).ap() for l in range(DEPTH)]
    S = Sched()
    dbg_outs = {}

    def dbg(name, ap, reads, dt=BF16):
        if not DBG:
            return
        shape = list(ap.shape)
        dd = nc.dram_tensor("dbg_" + name, shape, dt, kind="ExternalOutput").ap()
        dbg_outs[name] = dd
        S.op("sp", lambda e: e.dma_start(out=dd, in_=ap), reads=reads, dma="d_dbg_" + name)
        S.finals.append(("sp", [("d_dbg_" + name, 16)]))

    with ExitStack() as st:
        def sb(name, shape, dt):
            return st.enter_context(nc.sbuf_tensor(name, shape, dt))

        def psum(name, shape, dt):
            return st.enter_context(nc.psum_tensor(name, shape, dt))

        slots = [sb("slot%d" % i, [128, SLOTW], BF16) for i in range(NSLOT)]
        xTs = [sb("xTb%d" % i, [128, KC, T], F32) for i in range(2)]
        hT = sb("hT", [128, KC, T], BF16)
        acc = sb("acc", [128, KC, T], F32)
        QM = sb("QM", [128, KC, T], BF16)
        brs = [sb("br%d" % i, [128, 4, T], BF16) for i in range(2)]
        mgs = [sb("mgs%d" % i, [128, T], F32) for i in range(2)]
        sg = sb("sg", [128, 4, T], BF16)
        NF, NB = 7, 6
        f32s = [sb("f32s%d" % i, [128, 512], F32) for i in range(NF)]
        bfs = [sb("bfs%d" % i, [128, 512], BF16) for i in range(NB)]
        raws = [sb("raw%d" % i, [128, T + 4], BF16) for i in range(3)]
        dg = sb("dg", [128, 8, 4, 128], BF16)
        rnorm = sb("rnorm", [128, T], F32)
        qd = sb("qd", [128, 4, T], BF16)
        kT = sb("kT", [128, 4, T], BF16)
        vx = sb("vx", [128, NCH, 512], BF16)
        kdp = sb("kdp", [128, NCH, 4, 128], BF16)
        rstate = sb("rstate", [128, 256], F32)
        rstate_b = sb("rstate_b", [128, NV, 256], BF16)
        KTb = sb("KTb", [128, (NCH + 1) * 128], BF16)
        Vd = sb("Vd", [128, NCH + 1, 256], BF16)
        U = sb("U", [128, NCH + 1, 512], BF16)
        rawc = sb("rawc", [128, 8, 4], BF16)
        Btok = sb("Btok", [128, NCH, 256], BF16)
        xdp = sb("xdp", [128, NCH, 8, 128], BF16)
        acsT = sb("acsT", [8, T], F32)
        sm = sb("sm", [128, 8, 32], F32)
        sstate = sb("sstate", [128, 512], F32)
        sstate_b = sb("sstate_b", [128, NV, 512], BF16)
        cstf = sb("cstf_s", [128, NCF], F32)
        cstb = sb("cstb_s", [128, NCB], BF16)
        pcol = [sb("pcol_s%d" % l, [128, NPCOL], F32) for l in range(DEPTH)]
        prow = [sb("prow_s%d" % l, [128, NPROW], F32) for l in range(DEPTH)]
        poolw = [sb("poolw_s%d" % l, [128, 4, 128], BF16) for l in range(DEPTH)]
        modc = [sb("modc%d" % l, [128, 32], F32) for l in range(DEPTH)]
        sexp = [sb("sexp%d" % l, [128, 8], F32) for l in range(DEPTH)]
        ccol = sb("ccol_s", [128, 8], F32)
        scb = sb("scb", [128, 8], BF16)
        pbanks = [psum("pb%d" % i, [128, 512], F32) for i in range(7)]
        ptr = psum("ptr", [128, 1024], BF16)

        def CFv(name):
            o, n = FO[name]
            return cstf[:, o:o + n]

        def CBv(name):
            o, n = BO[name]
            return cstb[:, o:o + n]

        class Rot:
            def __init__(self, items, name):
                self.items, self.name, self.i = items, name, 0

            def next(self):
                k = self.i % len(self.items)
                self.i += 1
                return self.items[k], "%s%d" % (self.name, k)

        PS = Rot(pbanks, "ps")
        class RotPT:
            def __init__(self):
                self.i = 0

            def next(self):
                k = self.i % 2
                self.i += 1
                return ptr[:, k * 512:(k + 1) * 512], "ptr"
        PT = RotPT()
        FS = Rot(f32s, "fs")
        BS = Rot(bfs, "bs")
        RW = Rot(raws, "rw")
        MS = Rot(mgs, "ms")
        from collections import deque
        fillers = deque()

        def fill(k=1):
            for _ in range(k):
                if fillers:
                    ph = S.phase
                    fillers.popleft()[1]()
                    S.phase = ph

        def flush(upto=99):
            while fillers and fillers[0][0] <= upto:
                fill()

        def grp(fns):
            def f(e):
                last = None
                for fn in fns:
                    last = fn(e)
                return last
            f.nins = len(fns)
            return f

        def mm(out, lhsT, rhs, start=True, stop=True):
            return lambda e: e.matmul(out, lhsT=lhsT, rhs=rhs, start=start, stop=stop)

        def PE(fns, reads, writes):
            return S.op("pe", grp(fns), reads=reads, writes=writes)

        def ACT(out, in_, func, reads, writes, bias=None, scale=None):
            kw = {}
            if bias is not None:
                kw["bias"] = bias
            if scale is not None:
                kw["scale"] = scale
            return S.op("act", lambda e: e.activation(out=out, in_=in_, func=func, **kw), reads=reads, writes=writes)

        def TT(out, in0, in1, op, reads, writes, eng="dve"):
            return S.op(eng, lambda e: e.tensor_tensor(out=out, in0=in0, in1=in1, op=op), reads=reads, writes=writes)

        def TS(out, in0, s1, op0, reads, writes, s2=None, op1=None, eng="dve"):
            if op1 is None:
                return S.op(eng, lambda e: e.tensor_scalar(out=out, in0=in0, scalar1=s1, scalar2=0.0, op0=op0, op1=ALU.add), reads=reads, writes=writes)
            return S.op(eng, lambda e: e.tensor_scalar(out=out, in0=in0, scalar1=s1, scalar2=s2, op0=op0, op1=op1), reads=reads, writes=writes)

        def STT(out, in0, scalar, in1, op0, op1, reads, writes, eng="dve"):
            return S.op(eng, lambda e: e.scalar_tensor_tensor(out=out, in0=in0, scalar=scalar, in1=in1, op0=op0, op1=op1), reads=reads, writes=writes)

        def PRELOAD():
            return

        def RSQ(out, in_, eps_n, reads, writes):
            S.op("act", lambda e: e.activation(out=out, in_=in_, func=AF.Ln, bias=cst_eps[eps_n], scale=1.0 / eps_n), reads=reads, writes=writes)
            S.op("act", lambda e: e.activation(out=out, in_=out, func=AF.Exp, scale=-0.5), reads=writes, writes=writes)

        def CP(out, in_, reads, writes, eng="dve"):
            if eng == "act":
                return S.op("act", lambda e: e.copy(out=out, in_=in_), reads=reads, writes=writes)
            return S.op(eng, lambda e: e.tensor_copy(out=out, in_=in_), reads=reads, writes=writes)

        ring = {"n": 0}

        class Lazy:
            def __init__(self, l, name, first):
                self.a = (l, name, first); self.v = None

            def __getitem__(self, k):
                if self.v is None:
                    self.v = load_gran(*self.a)
                return self.v[k]

        def load_gran(l, name, first=True):
            o, n = woffs[name]
            k = ring["n"] % NSLOT
            ring["n"] += 1
            slot = slots[k]
            key = "slot%d" % k
            dkey = "wbf%d_%s" % (l, name)
            if first or name.startswith("ada"):
                src = wts_d[l][:, o:o + n]
                S.op("pool", lambda e: e.dma_start(out=slot[:, 0:n], in_=src), writes=[key], dma="d_ring%d" % k)
                if not name.startswith("ada"):
                    dst = wbf_d[l][:, o:o + n]
                    S.op("sp", lambda e: e.dma_start(out=dst, in_=slot[:, 0:n]), reads=[key], writes=[dkey], dma="d_wst%d" % k)
            else:
                src = wbf_d[l][:, o:o + n]
                S.op("sp", lambda e: e.dma_start(out=slot[:, 0:n], in_=src), reads=[dkey], writes=[key], dma="d_ring%d" % k)
            return slot, key, n

        S.op("sp", lambda e: e.dma_start(out=cstf[:], in_=cstf_d), writes=["cstf"], dma="d_c0")
        S.op("pool", lambda e: e.dma_start(out=cstb[:], in_=cstb_d), writes=["cstb"], dma="d_cb0")
        S.op("sp", lambda e: e.dma_start(out=ccol[:], in_=ccol_d), writes=["ccol"], dma="d_c1")
        for l in range(DEPTH):
            S.op("sp", (lambda l: lambda e: e.dma_start(out=pcol[l][:], in_=pcol_d[l]))(l), writes=["pcol%d" % l], dma="d_pc%d" % l)
            S.op("sp", (lambda l: lambda e: e.dma_start(out=prow[l][:], in_=prow_d[l]))(l), writes=["prow%d" % l], dma="d_pr%d" % l)
            S.op("pool", (lambda l: lambda e: e.dma_start(out=poolw[l][:].rearrange("p a b -> p (a b)"), in_=poolw_d[l]))(l),
                 writes=["poolw%d" % l], dma="d_pw%d" % l)
        ACT(scb[:], ccol[:], AF.Silu, ["ccol"], ["scb"])
        S.op("dve", lambda e: e.memset(kdp[:].rearrange("p a b c -> p (a b c)"), 0.0), writes=["kdp%d" % c for c in range(NCH)])
        S.op("dve", lambda e: e.memset(xdp[:].rearrange("p a b c -> p (a b c)"), 0.0), writes=["xdp%d" % c for c in range(NCH)])
        S.op("dve", lambda e: e.memset(rawc[:].rearrange("p a b -> p (a b)"), 0.0), writes=["rawc"])

        _eo = FO["epsc"][0]
        cst_eps = {D: cstf[:, _eo:_eo + 1], 128: cstf[:, _eo + 1:_eo + 2], 256: cstf[:, _eo + 2:_eo + 3]}
        onesb = CBv("onesb")
        ident = CBv("ident")
        tri = CFv("tri")
        onesf = CFv("ones")

        def layer_setup(l):
            pb, pk = PS.next()
            for gi in range(6):
                slot, key, n = load_gran(l, "ada%d" % gi)
                sv = slot[:, 0:4096].rearrange("p (k c) -> p k c", k=KC)
                fns = []
                for jj in range(4):
                    jt = gi * 4 + jj
                    for kc in range(KC):
                        fns.append(mm(pb[:, jt:jt + 1], sv[:, kc, jj * 128:(jj + 1) * 128], scb[:, kc:kc + 1], kc == 0, kc == KC - 1))
                PE(fns, [key, "scb"], [pk] if gi == 0 else [pk])
            o, n = PCOL["adab"]
            TT(modc[l][:, 0:24], pb[:, 0:24], pcol[l][:, o:o + n], ALU.add, [pk, "pcol%d" % l], ["modc%d" % l])
            o, n = PCOL["nw"]
            STT(modc[l][:, 24:32], modc[l][:, 8:16], 1.0, pcol[l][:, o:o + n], ALU.add, ALU.mult, ["modc%d" % l, "pcol%d" % l], ["modc%d" % l])
            o, n = PROW["sinks"]
            ACT(sexp[l][:], prow[l][:, o:o + n], AF.Exp, ["prow%d" % l], ["sexp%d" % l])
            o, n = PROW["alog"]
            ACT(prow[l][:, o:o + n], prow[l][:, o:o + n], AF.Exp, ["prow%d" % l], ["prow%d" % l])
            TS(prow[l][:, o:o + n], prow[l][:, o:o + n], -1.0, ALU.mult, ["prow%d" % l], ["prow%d" % l])

        for l in range(DEPTH):
            layer_setup(l)

        def proj_F(slot, key, c0, ntiles, evac):
            sv = slot[:, 0:4096].rearrange("p (k c) -> p k c", k=KC) if False else None
            for i in range(ntiles):
                pb, pk = PS.next()
                fns = []
                for kc in range(KC):
                    fns.append(mm(pb[:, 0:T], slotview(slot, kc, c0 + i * 128, 128), hT[:, kc, :], kc == 0, kc == KC - 1))
                PE(fns, [key, "hT"], [pk])
                evac(i, pb, pk)

        gw = {"w": 512}

        def slotview(slot, kc, c0, n):
            w = gw["w"]
            return slot[:, kc * w + c0: kc * w + c0 + n]

        def proj_T(slot, key, c0, ncols, evac):
            for c in range(NCH):
                pb, pk = PS.next()
                fns = []
                for kc in range(KC):
                    fns.append(mm(pb[:, 0:ncols], hT[:, kc, c * 128:(c + 1) * 128], slotview(slot, kc, c0, ncols), kc == 0, kc == KC - 1))
                PE(fns, [key, "hT"], [pk])
                evac(c, pb, pk)

        def merge(l, i, last, first, brt, brk):
            st_ = {}

            def step(jd):
                S.phase = "merge%d" % i
                hf = jd // 4
                if jd % 4 == 0:
                    st_["m"] = load_gran(l, "mg%d_%d" % (i, hf), first)
                    st_["u"] = load_gran(l, "up%d_%d" % (i, hf), first)
                mslot, mkey, _ = st_["m"]
                uslot, ukey, _ = st_["u"]
                pg_, pgk = PS.next()
                fns = [mm(pg_[:, 0:T], mslot[:, kc * 512 + (jd % 4) * 128: kc * 512 + (jd % 4 + 1) * 128], hT[:, kc, :], kc == 0, kc == KC - 1) for kc in range(KC)]
                PE(fns, [mkey, "hT"], [pgk])
                pu, puk = PS.next()
                fns = [mm(pu[:, 0:T], uslot[:, kc * 512 + (jd % 4) * 128: kc * 512 + (jd % 4 + 1) * 128], brt[:, kc, :], kc == 0, kc == 3) for kc in range(4)]
                PE(fns, [ukey, brk], [puk])
                sgm, sk_ = MS.next()
                ACT(sgm[:, 0:T], pg_[:, 0:T], AF.Sigmoid, [pgk], [sk_])
                if i == 0:
                    TT(acc[:, jd, :], pu[:, 0:T], sgm[:, 0:T], ALU.mult, [puk, sk_], ["acc%d" % jd])
                else:
                    TT(sgm[:, 0:T], pu[:, 0:T], sgm[:, 0:T], ALU.mult, [puk, sk_], [sk_])
                    if last:
                        TT(QM[:, jd, :], acc[:, jd, :], sgm[:, 0:T], ALU.add, ["acc%d" % jd, sk_], ["QM%d" % jd], eng="pool")
                    else:
                        TT(acc[:, jd, :], acc[:, jd, :], sgm[:, 0:T], ALU.add, ["acc%d" % jd, sk_], ["acc%d" % jd], eng="pool")
            for jd in range(KC):
                fillers.append((i, (lambda jd: lambda: step(jd))(jd)))

        def issue_x_load(gi):
            l, n = gi // NT, gi % NT
            if l >= DEPTH:
                return
            src = xT_d if l == 0 else xmid_d[l - 1]
            xb = xTs[gi % 2]
            xk = ["xT%d_%d" % (gi % 2, kc) for kc in range(KC)]
            srcv = src.rearrange("(k p) t -> p k t", p=128)[:, :, n * T:(n + 1) * T]
            S.op("pool", lambda e: e.dma_start(out=xb[:], in_=srcv), reads=["dram_x%d_%d" % (l, n)], writes=xk, dma="d_x%d" % (gi % 2))

        def tile_body(l, n):
            first = (n == 0)
            G0 = n * NCH
            gi = l * NT + n
            xT = xTs[gi % 2]
            dst = outT_d if l == DEPTH - 1 else xmid_d[l]
            xkeys = ["xT%d_%d" % (gi % 2, kc) for kc in range(KC)]
            dstv = dst.rearrange("(k p) t -> p k t", p=128)[:, :, n * T:(n + 1) * T]
            if n == 0:
                issue_x_load(gi)
            if (gi + 1) % NT != 0 or True:
                if (gi + 1) // NT == l:
                    issue_x_load(gi + 1)
            pc, pr = pcol[l], prow[l]
            pck, prk, mck = "pcol%d" % l, "prow%d" % l, "modc%d" % l

            S.phase = "norm"
            PRELOAD()
            pb, pk = PS.next()
            for kc in range(KC):
                sq, sqk = BS.next()
                ACT(sq[:, 0:T], xT[:, kc, :], AF.Square, [xkeys[kc]], [sqk])
                PE([mm(pb[:, 0:T], onesb, sq[:, 0:T], kc == 0, kc == KC - 1)], [sqk, "cstb"], [pk])
            r, rk = rnorm, "rnorm"
            if l == 0 and n == 0 and False:
                ss_, ssk = FS.next()
                CP(ss_[:, 0:T], pb[:, 0:T], [pk], [ssk])
                dbg("ssq", ss_[:, 0:T], [ssk], F32)
            RSQ(r[:, 0:T], pb[:, 0:T], D, [pk], [rk])
            pass
            for kc in range(KC):
                t_, tk = FS.next()
                STT(t_[:, 0:T], xT[:, kc, :], modc[l][:, 24 + kc:25 + kc], r[:, 0:T], ALU.mult, ALU.mult, [xkeys[kc], mck, rk], [tk])
                ACT(hT[:, kc, :], t_[:, 0:T], AF.Identity, [tk, mck], ["hT"], bias=modc[l][:, kc:kc + 1])

            S.phase = "A_proj"
            br, brk_ = brs[0], "br0"
            gw["w"] = 512
            sA_v = Lazy(l, "A_v", first)
            sA_qk = Lazy(l, "A_qk", first)
            sA_g = Lazy(l, "A_g", first)
            gw["w"] = 264
            sA_k = Lazy(l, "A_k", first)
            gw["w"] = 512
            qdm = CFv("qdm").rearrange("p (h t) -> p h t", h=4)

            def ev_qk(i, pb, pk):
                if i < 2:
                    for half in range(2):
                        h = 2 * i + half
                        TT(qd[:, h, :].rearrange("p (c t) -> p c t", c=NCH), pb[:, 0:T].rearrange("p (c t) -> p c t", c=NCH),
                           qdm[:, h:h + 1, :].to_broadcast([128, NCH, 128]), ALU.mult, [pk, "cstf"], ["qd%d" % h])
                else:
                    CP(kT[:, i - 2, :], pb[:, 0:T], [pk], ["kT%d" % (i - 2)], eng="act")
            proj_F(sA_qk[0], sA_qk[1], 0, 4, ev_qk)

            def ev_g(i, pb, pk):
                ACT(sg[:, i, :], pb[:, 0:T], AF.Silu, [pk], ["sg%d" % i])
            proj_F(sA_g[0], sA_g[1], 0, 4, ev_g)

            def ev_v(c, pb, pk):
                CP(vx[:, c, :], pb[:, 0:512], [pk], ["vx%d" % c], eng="act")
            proj_T(sA_v[0], sA_v[1], 0, 512, ev_v)
            kdec = CFv("kdec")

            def ev_k(c, pb, pk):
                pv = pb[:, 0:256].rearrange("p (j a d) -> p j a d", j=2, a=2)
                kv = kdp[:, c, :, :].rearrange("p (j a) x -> p j a x", a=2)
                kd4 = kdec.rearrange("p (j a) -> p j a", a=2)
                for a in range(2):
                    TT(kv[:, :, a, a * 64:(a + 1) * 64], pv[:, :, a, :], kd4[:, :, a:a + 1].to_broadcast([128, 2, 64]), ALU.mult,
                       [pk, "cstf"], ["kdp%d" % c])
            gw["w"] = 264
            proj_T(sA_k[0], sA_k[1], 0, 256, ev_k)
            S.phase = "D_dt"
            pb, pk = PS.next()
            fns = []
            for c in range(NCH):
                for kc in range(KC):
                    fns.append(mm(pb[:, c * 8:(c + 1) * 8], hT[:, kc, c * 128:(c + 1) * 128], slotview(sA_k[0], kc, 256, 8), kc == 0, kc == KC - 1))
            PE(fns, [sA_k[1], "hT"], [pk])
            o_db, _ = PROW["dtb"]
            o_al, _ = PROW["alog"]
            TT(sm[:, 6, :], pb[:, 0:32], pr[:, o_db:o_db + 32], ALU.add, [pk, prk], ["sm6"])
            ACT(sm[:, 6, :], sm[:, 6, :], AF.Exp, ["sm6"], ["sm6"])
            ACT(sm[:, 0, :], sm[:, 6, :], AF.Ln, ["sm6"], ["sm0"], bias=1.0)
            TT(sm[:, 1, :], sm[:, 0, :], pr[:, o_al:o_al + 32], ALU.mult, ["sm0", prk], ["sm1"])
            pb, pk = PS.next()
            PE([mm(pb[:, c * 8:(c + 1) * 8], tri, sm[:, 1, c * 8:(c + 1) * 8]) for c in range(NCH)] +
               [mm(pb[:, 32:64], onesf, sm[:, 1, :])], ["sm1", "cstf"], [pk])
            CP(sm[:, 2, :], pb[:, 0:32], [pk], ["sm2"])
            TT(sm[:, 6, :], pb[:, 32:64], sm[:, 2, :], ALU.subtract, [pk, "sm2"], ["sm6"])
            ACT(sm[:, 3, :], sm[:, 6, :], AF.Exp, ["sm6"], ["sm3"])
            ACT(sm[:, 4, :], pb[:, 32:64], AF.Exp, [pk], ["sm4"])
            TT(sm[:, 5, :], sm[:, 0, :], sm[:, 3, :], ALU.mult, ["sm0", "sm3"], ["sm5"])
            pb, pk = PS.next()
            PE([mm(pb[0:8, c * 128:(c + 1) * 128], sm[:, 1, c * 8:(c + 1) * 8], tri) for c in range(NCH)], ["sm1", "cstf"], [pk])
            CP(acsT[:, :], pb[0:8, 0:T], [pk], ["acsT"])
            gw["w"] = 512
            S.phase = "A_proj"
            S.phase = "A_state"
            gL = CFv("gL")
            for c in range(NCH):
                G = G0 + c
                pb, pk = PS.next()
                fns = []
                for j in range(2):
                    for a in range(2):
                        h = 2 * j + a
                        fns.append(mm(pb[:, j * 128:(j + 1) * 128], kdp[:, c, h, :], vx[:, c, h * 128:(h + 1) * 128], a == 0, a == 1))
                PE(fns, ["kdp%d" % c, "vx%d" % c], [pk])
                vnext = (G + 1) % NV
                if G == 0:
                    CP(rstate[:], pb[:, 0:256], [pk], ["rstate"])
                else:
                    for j in range(2):
                        STT(rstate[:, j * 128:(j + 1) * 128], rstate[:, j * 128:(j + 1) * 128], gL[:, j:j + 1], pb[:, j * 128:(j + 1) * 128],
                            ALU.mult, ALU.add, ["rstate", pk, "cstf"], ["rstate"])
                CP(rstate_b[:, vnext, :], rstate[:], ["rstate"], ["rsb%d" % vnext])
            maskTA = CFv("maskTA")
            sc_ps = {}

            def A_scores(c):
                pb, pk = PS.next()
                fns = []
                for h in range(4):
                    fns.append(mm(pb[:, h * 128:(h + 1) * 128], kT[:, h // 2, c * 128:(c + 1) * 128], qd[:, h, c * 128:(c + 1) * 128]))
                PE(fns, ["kT0", "kT1"] + ["qd%d" % h for h in range(4)], [pk])
                stt, stk = BS.next()
                TT(stt[:, :], pb[:, :], maskTA, ALU.mult, [pk, "cstf"], [stk])
                sc_ps[c] = (stt, stk)

            def A_out(c):
                G = G0 + c
                stt, stk = sc_ps[c]
                pb, pk = PS.next()
                fns = []
                ver = G % NV
                for h in range(4):
                    fns.append(mm(pb[:, h * 128:(h + 1) * 128], vx[:, c, h * 128:(h + 1) * 128], stt[:, h * 128:(h + 1) * 128], True, G == 0))
                    if G > 0:
                        fns.append(mm(pb[:, h * 128:(h + 1) * 128], rstate_b[:, ver, (h // 2) * 128:(h // 2 + 1) * 128], qd[:, h, c * 128:(c + 1) * 128], False, True))
                PE(fns, ["vx%d" % c, stk, "rsb%d" % ver] + ["qd%d" % h for h in range(4)], [pk])
                sq, sqk = BS.next()
                ACT(sq[:, :], pb[:, :], AF.Square, [pk], [sqk])
                p2, p2k = PS.next()
                PE([mm(p2[:, :], onesb, sq[:, :])], [sqk, "cstb"], [p2k])
                r, rk = FS.next()
                RSQ(r[:, :], p2[:, :], 128, [p2k], [rk])
                TT(r[:, :], pb[:, :], r[:, :], ALU.mult, [pk, rk], [rk])
                TT(br[:, :, c * 128:(c + 1) * 128], r[:, :].rearrange("p (h t) -> p h t", h=4),
                   sg[:, :, c * 128:(c + 1) * 128], ALU.mult, [rk] + ["sg%d" % i for i in range(4)], [brk_])
            S.phase = "A_core"
            PRELOAD()
            A_scores(0)
            for c in range(NCH):
                if c + 1 < NCH:
                    A_scores(c + 1)
                A_out(c)
            if l == 0: dbg("brA%d" % n, br[:], [brk_])
            if l == 0: dbg("hT%d" % n, hT[:], ["hT"])
            if l == 0 and n == 0: dbg("modc", modc[l][:], [mck], F32)
            merge(l, 0, False, first, br, brk_)

            S.phase = "B_proj"
            br, brk_ = brs[1], "br1"
            gw["w"] = 512
            sB_q = Lazy(l, "B_q", first)
            gw["w"] = 384
            sB_kv = Lazy(l, "B_kv", first)
            gw["w"] = 512
            sB_g = Lazy(l, "B_g", first)
            rm8 = CFv("rowmask8")

            def ev_q(i, pb, pk):
                for g in range(2):
                    TS(QM[:, g * 4 + i, :], pb[:, 0:T], rm8[:, g:g + 1], ALU.mult, [pk, "cstf"], ["QM%d" % (g * 4 + i)])
            proj_F(sB_q[0], sB_q[1], 0, 4, ev_q)
            gw["w"] = 384

            def ev_kt(i, pb, pk):
                CP(KTb[:, 128:128 + T], pb[:, 0:T], [pk], ["KTb"], eng="act")
            proj_F(sB_kv[0], sB_kv[1], 0, 1, ev_kt)

            def ev_vd(c, pb, pk):
                CP(Vd[:, c + 1, :], pb[:, 0:256], [pk], ["Vd%d" % (c + 1)], eng="act")
            proj_T(sB_kv[0], sB_kv[1], 128, 256, ev_vd)
            gw["w"] = 512
            proj_F(sB_g[0], sB_g[1], 0, 4, ev_g)
            S.phase = "B_core"
            swadec = CBv("swadec").rearrange("p (g k x) -> p g k x", g=2, k=2)
            b_state = {}

            def B_s1(c, g):
                G = G0 + c
                kbs = [1] if G == 0 else [0, 1]
                ets = []
                for kb in kbs:
                    pb, pk = PS.next()
                    fns = [mm(pb[:, jj * 128:(jj + 1) * 128], KTb[:, (c + kb) * 128:(c + kb + 1) * 128], QM[:, g * 4 + jj, c * 128:(c + 1) * 128])
                           for jj in range(4)]
                    PE(fns, ["KTb"] + ["QM%d" % (g * 4 + jj) for jj in range(4)], [pk])
                    ex, exk = FS.next()
                    ACT(ex[:, :], pb[:, :], AF.Exp, [pk], [exk])
                    et, etk = BS.next()
                    TT(et[:, :], ex[:, :], swadec[:, g, kb, :], ALU.mult, [exk, "cstb"], [etk], eng="pool")
                    ets.append((et, etk, kb))
                b_state[(c, g)] = ets

            def B_s2(c, g):
                ets = b_state.pop((c, g))
                pden, pdk = PS.next()
                PE([mm(pden[:, :], onesb, et[:, :], i == 0, i == len(ets) - 1) for i, (et, etk, kb) in enumerate(ets)],
                   [e_[1] for e_ in ets] + ["cstb"], [pdk])
                ppv, ppk = PS.next()
                PE([mm(ppv[:, :], Vd[:, c + kb, g * 128:(g + 1) * 128], et[:, :], i == 0, i == len(ets) - 1) for i, (et, etk, kb) in enumerate(ets)],
                   [e_[1] for e_ in ets] + ["Vd%d" % (c + kb) for (_, _, kb) in ets], [ppk])
                t1, t1k = FS.next()
                for jj in range(4):
                    ACT(t1[:, jj * 128:(jj + 1) * 128], pden[:, jj * 128:(jj + 1) * 128], AF.Ln, [pdk, "sexp%d" % l], [t1k],
                        bias=sexp[l][:, g * 4 + jj:g * 4 + jj + 1])
                ACT(t1[:, :], t1[:, :], AF.Exp, [t1k], [t1k], scale=-1.0)
                TT(t1[:, :], ppv[:, :], t1[:, :], ALU.mult, [ppk, t1k], [t1k])
                lo, hi = g * 64, (g + 1) * 64
                TT(br[lo:hi, :, c * 128:(c + 1) * 128], t1[lo:hi, :].rearrange("p (j q) -> p j q", j=4), sg[lo:hi, :, c * 128:(c + 1) * 128],
                   ALU.mult, [t1k] + ["sg%d" % i for i in range(4)], [brk_])
            its = [(c, g) for c in range(NCH) for g in range(2)]
            PRELOAD()
            B_s1(*its[0])
            for i_, it in enumerate(its):
                if i_ + 1 < len(its):
                    B_s1(*its[i_ + 1])
                B_s2(*it)
                fill(1)
            CP(KTb[:, 0:128], KTb[:, NCH * 128:(NCH + 1) * 128], ["KTb"], ["KTb"])
            CP(Vd[:, 0, :], Vd[:, NCH, :], ["Vd%d" % NCH], ["Vd0"])
            if l == 0: dbg("brB%d" % n, br[:], [brk_])
            merge(l, 1, False, first, br, brk_)

            S.phase = "C_proj"
            br, brk_ = brs[0], "br0"
            sC_u = Lazy(l, "C_u", first)
            sC_g = Lazy(l, "C_g", first)

            def ev_u(c, pb, pk):
                CP(U[:, c + 1, :], pb[:, 0:512], [pk], ["U%d" % (c + 1)], eng="act")
            proj_T(sC_u[0], sC_u[1], 0, 512, ev_u)
            proj_F(sC_g[0], sC_g[1], 0, 4, ev_g)
            flush(0)
            S.phase = "C_core"
            poolM = CBv("poolM").rearrange("p (k i t) -> p k i t", k=3, i=4)
            o_ps, _ = PCOL["pscale"]
            for c in range(NCH):
                G = G0 + c
                pb, pk = PS.next()
                fns = []
                for i in range(4):
                    if G == 0:
                        fns.append(mm(pb[:, i * 128:(i + 1) * 128], U[:, c + 1, i * 128:(i + 1) * 128], poolM[:, 2, i, :]))
                    else:
                        fns.append(mm(pb[:, i * 128:(i + 1) * 128], U[:, c, i * 128:(i + 1) * 128], poolM[:, 1, i, :], True, False))
                        fns.append(mm(pb[:, i * 128:(i + 1) * 128], U[:, c + 1, i * 128:(i + 1) * 128], poolM[:, 0, i, :], False, True))
                PE(fns, ["U%d" % c, "U%d" % (c + 1), "cstb"], [pk])
                dT_, dk_ = BS.next()
                CP(dT_[:, :], pb[:, :], [pk], [dk_], eng="act")
                p2, p2k = PS.next()
                PE([mm(p2[:, i * 128:(i + 1) * 128], poolw[l][:, i, :], dT_[:, i * 128:(i + 1) * 128]) for i in range(4)], [dk_, "poolw%d" % l], [p2k])
                for i in range(4):
                    STT(br[:, i, c * 128:(c + 1) * 128], p2[:, i * 128:(i + 1) * 128], pc[:, o_ps + i:o_ps + i + 1], sg[:, i, c * 128:(c + 1) * 128],
                        ALU.mult, ALU.mult, [p2k, pck, "sg%d" % i], [brk_])
                fill(1)
            CP(U[:, 0, :], U[:, NCH, :], ["U%d" % NCH], ["U0"])
            if l == 0: dbg("brC%d" % n, br[:], [brk_])
            merge(l, 2, False, first, br, brk_)

            S.phase = "D_proj"
            br, brk_ = brs[1], "br1"
            sD_x0 = Lazy(l, "D_x0", first)
            sD_x1 = Lazy(l, "D_x1", first)
            sD_z = Lazy(l, "D_z", first)
            o_cw, _ = PCOL["convw"]
            o_cb, _ = PCOL["convb"]

            if first:
                for i in range(8):
                    for k in range(4):
                        TS(dg[:, i, k, :], ident, pc[:, o_cw + i * 4 + k:o_cw + i * 4 + k + 1], ALU.mult, ["cstb", pck], ["dg"])
            pend = []

            def conv_finish(i, rw, rwk):
                pcv, pcvk = PS.next()
                PE([mm(pcv[:, 0:T], dg[:, i, k, :], rw[:, k:k + T], k == 0, k == 3) for k in range(4)], ["dg", rwk], [pcvk])
                if i < 4:
                    ACT(qd[:, i, :], pcv[:, 0:T], AF.Silu, [pcvk, pck], ["qd%d" % i], bias=pc[:, o_cb + i:o_cb + i + 1])
                else:
                    ACT(kT[:, i - 4, :], pcv[:, 0:T], AF.Silu, [pcvk, pck], ["kT%d" % (i - 4)], bias=pc[:, o_cb + i:o_cb + i + 1])

            def mk_ev_xbc(base):
                def ev(i0, pb, pk):
                    i = base + i0
                    rw, rwk = RW.next()
                    if first:
                        S.op("dve", lambda e: e.memset(rw[:, 0:3], 0.0), writes=[rwk])
                    else:
                        CP(rw[:, 0:3], rawc[:, i, 0:3], ["rawc"], [rwk])
                    CP(rw[:, 3:3 + T], pb[:, 0:T], [pk, rwk], [rwk], eng="act")
                    CP(rawc[:, i, 0:3], rw[:, T:T + 3], [rwk], ["rawc"])
                    if pend:
                        conv_finish(*pend.pop())
                    pend.append((i, rw, rwk))
                    fill(1)
                return ev
            proj_F(sD_x0[0], sD_x0[1], 0, 4, mk_ev_xbc(0))
            proj_F(sD_x1[0], sD_x1[1], 0, 4, mk_ev_xbc(4))
            conv_finish(*pend.pop())
            proj_F(sD_z[0], sD_z[1], 0, 4, ev_g)
            sel = CFv("sel").rearrange("p (h m) -> p h m", h=12)
            S.phase = "D_tr"
            for c in range(NCH):
                pt, ptk = PT.next()
                S.op("pe", grp([(lambda j, pt, c: lambda e: e.transpose(pt[:, j * 128:(j + 1) * 128], qd[:, j, c * 128:(c + 1) * 128], ident))(j, pt, c) for j in range(4)]),
                     reads=["qd%d" % j for j in range(4)] + ["cstb"], writes=[ptk])
                pv = pt[:, 0:512].rearrange("p (j a d) -> p j a d", j=4, a=2)
                xv = xdp[:, c, :, :].rearrange("p (j a) x -> p j a x", a=2)
                dtv = sm[:, 0, c * 8:(c + 1) * 8].rearrange("p (j a) -> p j a", a=2)
                for a in range(2):
                    TT(xv[:, :, a, a * 64:(a + 1) * 64], pv[:, :, a, :], dtv[:, :, a:a + 1].to_broadcast([128, 4, 64]), ALU.mult,
                       [ptk, "sm0"], ["xdp%d" % c])
                TT(vx[:, c, :].rearrange("p (h d) -> p h d", h=8), pt[:, 0:512].rearrange("p (h d) -> p h d", h=8),
                   sm[:, 5, c * 8:(c + 1) * 8].unsqueeze(2).to_broadcast([128, 8, 64]), ALU.mult, [ptk, "sm5"], ["vx%d" % c])
                pt2, pt2k = PT.next()
                S.op("pe", grp([(lambda g, pt2, c: lambda e: e.transpose(pt2[:, g * 128:(g + 1) * 128], kT[:, g, c * 128:(c + 1) * 128], ident))(g, pt2, c) for g in range(2)]),
                     reads=["kT0", "kT1", "cstb"], writes=[pt2k])
                CP(Btok[:, c, :], pt2[:, 0:256], [pt2k], ["Btok%d" % c])
            S.phase = "D_state"
            for c in range(NCH):
                G = G0 + c
                pb, pk = PS.next()
                PE([mm(pb[:, g * 256:(g + 1) * 256], Btok[:, c, g * 128:(g + 1) * 128], vx[:, c, g * 256:(g + 1) * 256]) for g in range(2)],
                   ["Btok%d" % c, "vx%d" % c], [pk])
                vnext = (G + 1) % NV
                if G == 0:
                    CP(sstate[:], pb[:, :], [pk], ["sstate"])
                else:
                    TT(sstate[:].rearrange("p (h d) -> p h d", h=8), sstate[:].rearrange("p (h d) -> p h d", h=8),
                       sm[:, 4, c * 8:(c + 1) * 8].unsqueeze(2).to_broadcast([128, 8, 64]), ALU.mult, ["sstate", "sm4"], ["sstate"])
                    TT(sstate[:], sstate[:], pb[:, :], ALU.add, ["sstate", pk], ["sstate"])
                CP(sstate_b[:, vnext, :], sstate[:], ["sstate"], ["ssb%d" % vnext])
            flush(1)
            S.phase = "D_core"
            o_dk, _ = PCOL["dskip"]
            o_sw, _ = PCOL["ssmw"]
            d_state = {}

            def D_s1(c):
                pcb, pcbk = PS.next()
                PE([mm(pcb[:, g * 128:(g + 1) * 128], kT[:, g, c * 128:(c + 1) * 128], kT[:, 2 + g, c * 128:(c + 1) * 128]) for g in range(2)],
                   ["kT%d" % i for i in range(4)], [pcbk])
                cbm, cbk = FS.next()
                TT(cbm[:, 0:256].rearrange("p (g t) -> p g t", g=2), pcb[:, 0:256].rearrange("p (g t) -> p g t", g=2),
                   tri.unsqueeze(1).to_broadcast([128, 2, 128]), ALU.mult, [pcbk, "cstf"], [cbk])
                gts = []
                for g in range(2):
                    sgt, sgk = FS.next()
                    pbc, pbck = PS.next()
                    PE([mm(pbc[:, hh * 128:(hh + 1) * 128], sel[0:8, g * 4 + hh, :], acsT[:, c * 128:(c + 1) * 128]) for hh in range(4)], ["acsT", "cstf"], [pbck])
                    for hh in range(4):
                        h = g * 4 + hh
                        TS(sgt[:, hh * 128:(hh + 1) * 128], pbc[:, hh * 128:(hh + 1) * 128], sm[:, 2, c * 8 + h:c * 8 + h + 1], ALU.subtract, [pbck, "sm2"], [sgk],
                           s2=0.0, op1=ALU.min)
                    ACT(sgt[:, :], sgt[:, :], AF.Exp, [sgk], [sgk])
                    gt, gtk = BS.next()
                    TT(gt[:, :].rearrange("p (h t) -> p h t", h=4), sgt[:, :].rearrange("p (h t) -> p h t", h=4),
                       cbm[:, g * 128:(g + 1) * 128].unsqueeze(1).to_broadcast([128, 4, 128]), ALU.mult, [sgk, cbk], [gtk], eng="pool")
                    gts.append((gt, gtk))
                d_state[c] = gts

            def D_s2(c):
                G = G0 + c
                ver = G % NV
                gts = d_state.pop(c)
                pd, pdk = PS.next()
                fns = []
                for j in range(4):
                    g = j // 2
                    for a_ in range(2):
                        h = 2 * j + a_
                        fns.append(mm(pd[:, j * 128:(j + 1) * 128], xdp[:, c, h, :], gts[g][0][:, (h % 4) * 128:(h % 4 + 1) * 128], a_ == 0, a_ == 1))
                PE(fns, ["xdp%d" % c, gts[0][1], gts[1][1]], [pdk])
                y, yk = FS.next()
                if G > 0:
                    po, pok = PS.next()
                    PE([mm(po[:, j * 128:(j + 1) * 128], sstate_b[:, ver, j * 128:(j + 1) * 128], kT[:, 2 + j // 2, c * 128:(c + 1) * 128]) for j in range(4)],
                       ["ssb%d" % ver, "kT2", "kT3"], [pok])
                    pe_, pek = PS.next()
                    PE([mm(pe_[:, j * 128:(j + 1) * 128], sel[0:8, 8 + j, :], acsT[:, c * 128:(c + 1) * 128]) for j in range(4)], ["acsT", "cstf"], [pek])
                    ep, epk = FS.next()
                    ACT(ep[:, :], pe_[:, :], AF.Exp, [pek], [epk])
                    TT(ep[:, :], po[:, :], ep[:, :], ALU.mult, [pok, epk], [epk])
                    TT(y[:, :], pd[:, :], ep[:, :], ALU.add, [pdk, epk], [yk])
                else:
                    CP(y[:, :], pd[:, :], [pdk], [yk])
                for j in range(4):
                    STT(y[:, j * 128:(j + 1) * 128], qd[:, j, c * 128:(c + 1) * 128], pc[:, o_dk + j:o_dk + j + 1], y[:, j * 128:(j + 1) * 128],
                        ALU.mult, ALU.add, ["qd%d" % j, pck, yk], [yk])
                TT(y[:, :].rearrange("p (j t) -> p j t", j=4), y[:, :].rearrange("p (j t) -> p j t", j=4), sg[:, :, c * 128:(c + 1) * 128],
                   ALU.mult, [yk] + ["sg%d" % i for i in range(4)], [yk], eng="pool")
                sq, sqk = BS.next()
                ACT(sq[:, :], y[:, :], AF.Square, [yk], [sqk])
                p2, p2k = PS.next()
                PE([mm(p2[:, g * 128:(g + 1) * 128], onesb, sq[:, (2 * g + a_) * 128:(2 * g + a_ + 1) * 128], a_ == 0, a_ == 1) for g in range(2) for a_ in range(2)],
                   [sqk, "cstb"], [p2k])
                r, rk = FS.next()
                RSQ(r[:, 0:256], p2[:, 0:256], 256, [p2k], [rk])
                for j in range(4):
                    g = j // 2
                    STT(br[:, j, c * 128:(c + 1) * 128], y[:, j * 128:(j + 1) * 128], pc[:, o_sw + j:o_sw + j + 1], r[:, g * 128:(g + 1) * 128],
                        ALU.mult, ALU.mult, [yk, pck, rk], [brk_])
            PRELOAD()
            D_s1(0)
            for c in range(NCH):
                if c + 1 < NCH:
                    D_s1(c + 1)
                fill(1)
                D_s2(c)
                fill(1)
            if l == 0: dbg("brD%d" % n, br[:], [brk_])
            merge(l, 3, True, first, br, brk_)
            if l == 0: dbg("mrg%d" % n, QM[:], ["QM%d" % k_ for k_ in range(KC)])

            flush()
            S.phase = "outproj"
            wo = [Lazy(l, "wo_0", first), Lazy(l, "wo_1", first)]
            lastl = (l == DEPTH - 1)
            for jd in range(KC):
                wslot, wkey = wo[jd // 4][0], wo[jd // 4][1]
                pb, pk = PS.next()
                PE([mm(pb[:, 0:T], slotview(wslot, kc, (jd % 4) * 128, 128), QM[:, kc, :], kc == 0, kc == KC - 1) for kc in range(KC)],
                   [wkey] + ["QM%d" % kc for kc in range(KC)], [pk])
                STT(xT[:, jd, :], pb[:, 0:T], modc[l][:, 16 + jd:17 + jd], xT[:, jd, :], ALU.mult, ALU.add, [pk, mck, xkeys[jd]], [xkeys[jd]])
            if lastl:
                PRELOAD()
                pb, pk = PS.next()
                for kc in range(KC):
                    sq, sqk = BS.next()
                    ACT(sq[:, 0:T], xT[:, kc, :], AF.Square, [xkeys[kc]], [sqk])
                    PE([mm(pb[:, 0:T], onesb, sq[:, 0:T], kc == 0, kc == KC - 1)], [sqk, "cstb"], [pk])
                r, rk = FS.next()
                RSQ(r[:, 0:T], pb[:, 0:T], D, [pk], [rk])
                o_fn, _ = PCOL["fnw"]
                for kc in range(KC):
                    STT(xT[:, kc, :], xT[:, kc, :], pc[:, o_fn + kc:o_fn + kc + 1], r[:, 0:T], ALU.mult, ALU.mult, [xkeys[kc], pck, rk], [xkeys[kc]])
            S.op("pool", lambda e: e.dma_start(out=dstv, in_=xT[:]), reads=xkeys,
                 writes=["dram_x%d_%d" % (l + 1, n)], dma="d_o%d" % (gi % 2))

        for l in range(DEPTH):
            for n in range(NT):
                tile_body(l, n)
        S.finals.append(("pool", [("d_o0", S.cnt["d_o0"]), ("d_o1", S.cnt["d_o1"])]))
        print("sbuf bytes remaining", nc.sbuf_bytes_remaining)
        S.emit(nc)
    global LAST_SCHED
    LAST_SCHED = S
    return nc


_PROG = {}
DBG = False


def kernel(**inputs):
    inp = {k: np.asarray(v) for k, v in inputs.items()}
    x = inp["x"]
    B, NTOK, _ = x.shape
    DEPTH = inp["w_in"].shape[0]
    key = (NTOK, DEPTH)
    if key not in _PROG:
        _PROG[key] = build_program(NTOK, DEPTH)
    nc = _PROG[key]
    cf, _, cb, _ = make_consts()
    shared = {"cstf": cf, "cstb": cb}
    for l in range(DEPTH):
        w, _ = host_weights(inp, l)
        pc, pr, pw = host_small(inp, l)
        shared["wts%d" % l] = w
        shared["pcol%d" % l] = pc
        shared["prow%d" % l] = pr
        shared["poolw%d" % l] = pw
    in_maps = []
    for b in range(B):
        m = dict(shared)
        m["xT"] = np.ascontiguousarray(x[b].T)
        m["ccol"] = np.ascontiguousarray(inp["c"][b].reshape(8, 128).T)
        in_maps.append(m)
    res = run_bass_kernel_spmd(nc, in_maps, core_ids=list(range(B)))
    if DBG:
        global DBG_RES
        DBG_RES = res.results
    out = np.stack([np.ascontiguousarray(r["outT"].T) for r in res.results], 0)
    return out.astype(np.float32)
```

```python
import numpy as np
from contextlib import ExitStack
import concourse.bass as bass
import concourse.mybir as mybir
from concourse.bass_utils import run_bass_kernel_spmd

F32 = mybir.dt.float32
BF16 = mybir.dt.bfloat16
AF = mybir.ActivationFunctionType
ALU = mybir.AluOpType

D = 1024
T = 512
NCH = 4
KC = 8
NV = NCH + 1
EPS = 1e-6
NSLOT = 3
SLOTW = 4096
ENGS = ("pe", "act", "dve", "pool", "sp")

O_RQ, O_RK, O_RV, O_RG = 0, 256, 512, 1024
O_SQ, O_SK, O_SV, O_SG = 1536, 2048, 2176, 2304
O_PX, O_PG = 2816, 3328
O_XBC, O_MZ, O_MDT, O_MG = 3840, 4864, 5376, 5384


class Sched:
    def __init__(self):
        self.ops = {e: [] for e in ENGS}
        self.cnt = {}
        self.waited = {e: {} for e in ENGS}
        self.res = {}
        self.sem_keys = []
        self.finals = []
        self.phase = "setup"
        self.pe_tags = []

    def _sem(self, k):
        if k not in self.cnt:
            self.cnt[k] = 0
            self.sem_keys.append(k)

    def op(self, eng, fn, reads=(), writes=(), dma=None):
        need = {}

        def want(tok):
            if tok is None:
                return
            s, v = tok
            if s == "c_" + eng and eng == "pe":
                return
            if need.get(s, 0) < v:
                need[s] = v

        for r in reads:
            st = self.res.get(r)
            if st is not None:
                want(st[0])
                if r.startswith("ps") or r.startswith("pt"):
                    for s_, v_ in st[1].items():
                        want((s_, v_))
        for w in writes:
            st = self.res.get(w)
            if st is not None:
                want(st[0])
                for s, v in st[1].items():
                    want((s, v))
        waits = []
        wd = self.waited[eng]
        for s, v in need.items():
            if wd.get(s, 0) < v:
                wd[s] = v
                waits.append((s, v))
        if dma is not None:
            k, inc = dma, 16
        else:
            k, inc = "c_" + eng, 1
        self._sem(k)
        self.cnt[k] += inc
        tok = (k, self.cnt[k])
        if eng == "pe":
            self.pe_tags.append((self.phase, getattr(fn, "nins", 1)))
        self.ops[eng].append((waits, fn, k, inc))
        for r in reads:
            st = self.res.setdefault(r, [None, {}])
            if st[1].get(k, 0) < tok[1]:
                st[1][k] = tok[1]
        for w in writes:
            self.res[w] = [tok, {}]
        return tok

    def emit(self, nc):
        with ExitStack() as st:
            sems = {k: st.enter_context(nc.semaphore(k)) for k in self.sem_keys}
            block = st.enter_context(nc.Block())
            finals = {}
            for eng, toks in self.finals:
                finals.setdefault(eng, []).extend(toks)

            def mk(ename):
                def body(e):
                    for waits, fn, k, inc in self.ops[ename]:
                        for s, v in waits:
                            e.wait_ge(sems[s], v)
                        fn(e).then_inc(sems[k], inc)
                    for s, v in finals.get(ename, []):
                        e.wait_ge(sems[s], v)
                return body

            block.sync(mk("sp"))
            block.tensor(mk("pe"))
            block.scalar(mk("act"))
            block.vector(mk("dve"))
            block.gpsimd(mk("pool"))


def _gran_cols():
    ar = np.arange
    perm_att = np.concatenate([np.concatenate([jj * 64 + ar(64), (4 + jj) * 64 + ar(64)]) for jj in range(4)])
    g = {}
    g["A_v"] = O_RV + ar(512)
    g["A_qk"] = np.concatenate([O_RQ + ar(256), O_RK + ar(256)])
    g["A_g"] = O_RG + ar(512)
    g["A_k"] = np.concatenate([O_RK + ar(256), O_MDT + ar(8)])
    g["B_q"] = O_SQ + perm_att
    g["B_kv"] = np.concatenate([O_SK + ar(128), O_SV + np.concatenate([ar(64), ar(64), 64 + ar(64), 64 + ar(64)])])
    g["B_g"] = O_SG + perm_att
    g["C_u"] = O_PX + ar(512)
    g["C_g"] = O_PG + ar(512)
    g["D_x0"] = O_XBC + ar(512)
    g["D_x1"] = O_XBC + 512 + ar(512)
    g["D_z"] = O_MZ + ar(512)
    for i in range(4):
        g["mg%d_0" % i] = O_MG + i * 1024 + ar(512)
        g["mg%d_1" % i] = O_MG + i * 1024 + 512 + ar(512)
    return g, perm_att


STREAM = []
for _i, (_m, _gs) in enumerate([("A", ["A_v", "A_qk", "A_g", "A_k"]), ("B", ["B_q", "B_kv", "B_g"]),
                                ("C", ["C_u", "C_g"]), ("D", ["D_x0", "D_x1", "D_z"])]):
    STREAM += _gs + ["mg%d_0" % _i, "up%d_0" % _i, "mg%d_1" % _i, "up%d_1" % _i]
STREAM += ["wo_0", "wo_1"]
ADA = ["ada%d" % i for i in range(6)]


def _kc_layout(w):
    n = w.shape[1]
    return np.ascontiguousarray(w.reshape(KC, 128, n).transpose(1, 0, 2)).reshape(128, KC * n)


def host_weights(inp, l):
    gcols, perm_att = _gran_cols()
    W = inp["w_in"][l]
    parts, offs, o = [], {}, 0
    for name in ADA + STREAM:
        if name.startswith("ada"):
            i = int(name[3:])
            a = _kc_layout(inp["ada_w"][l][:, i * 512:(i + 1) * 512])
        elif name.startswith("up"):
            i = int(name[2]); hf = int(name[4])
            wu = inp["w_up"][l][i]
            if i == 1:
                wu = wu[perm_att]
            wu = wu[:, hf * 512:(hf + 1) * 512]
            a = np.ascontiguousarray(wu.reshape(4, 128, 512).transpose(1, 0, 2)).reshape(128, 2048)
        elif name.startswith("wo"):
            i = int(name[3:])
            a = _kc_layout(inp["w_out"][l][:, i * 512:(i + 1) * 512])
        else:
            a = _kc_layout(W[:, gcols[name]])
        offs[name] = (o, a.shape[1])
        o += a.shape[1]
        parts.append(a)
    return np.ascontiguousarray(np.concatenate(parts, axis=1), dtype=np.float32), offs


def _weight_offsets():
    gcols, _ = _gran_cols()
    offs, o = {}, 0
    for name in ADA + STREAM:
        if name.startswith("ada") or name.startswith("wo"):
            n = 4096
        elif name.startswith("up"):
            n = 2048
        else:
            n = KC * len(gcols[name])
        offs[name] = (o, n)
        o += n
    return offs, o


CF = {}
CB = {}


def make_consts():
    p = np.arange(128)
    s = np.arange(128)[:, None].astype(np.float64)
    l = np.arange(128)[None, :].astype(np.float64)
    gam = 1.0 - 2.0 ** (-5.0 - np.arange(4))
    rowmask = np.stack([(p < 64), (p >= 64)], 1).astype(np.float64)
    f = {}
    f["rowmask8"] = rowmask / 8.0
    f["qdm"] = np.stack([rowmask[:, h % 2][:, None] * gam[h] ** (np.arange(128) + 1.0)[None, :] for h in range(4)], 1).reshape(128, 512)
    f["maskTA"] = np.stack([(s <= l) * gam[h] ** (-(s + 1.0)) / 8.0 for h in range(4)], 1).reshape(128, 512)
    f["kdec"] = np.stack([gam[h] ** (127.0 - np.arange(128)) / 8.0 for h in range(4)], 1)
    f["gL"] = np.stack([np.where(p < 64, gam[2 * j] ** 128, gam[2 * j + 1] ** 128) for j in range(2)], 1)
    f["tri"] = (s <= l).astype(np.float64)
    sel = np.zeros((128, 12, 128))
    for h in range(8):
        sel[h, h, :] = 1.0
    for j in range(4):
        sel[2 * j, 8 + j, :64] = 1.0
        sel[2 * j + 1, 8 + j, 64:] = 1.0
    f["sel"] = sel.reshape(128, 12 * 128)
    f["ones"] = np.ones((128, 128))
    f["epsc"] = np.tile(np.array([EPS, EPS, EPS])[None, :], (128, 1))
    b = {}
    slopes = 2.0 ** (-(np.arange(8) + 1.0))
    k = np.arange(128)[:, None].astype(np.float64)
    q = np.arange(128)[None, :].astype(np.float64)
    dec = np.zeros((128, 2, 2, 4, 128))
    for g in range(2):
        for jj in range(4):
            sl = slopes[4 * g + jj]
            dec[:, g, 0, jj, :] = np.where(k > q, np.exp(-sl * (128.0 + q - k)), 0.0)
            dec[:, g, 1, jj, :] = np.where(k <= q, np.exp(-sl * (q - k)), 0.0)
    b["swadec"] = dec.reshape(128, 2048)
    M = np.zeros((128, 3, 4, 128))
    for i, w in enumerate((2, 4, 8, 16)):
        dlt = q - k
        M[:, 0, i, :] = (1.0 / w) * ((dlt >= 0) & (dlt <= w - 1)) - (dlt == 0)
        M[:, 1, i, :] = (1.0 / w) * ((q - (k - 128.0)) <= w - 1)
        cnt = np.minimum(q + 1.0, w)
        M[:, 2, i, :] = (1.0 / cnt) * ((dlt >= 0) & (dlt <= w - 1)) - (dlt == 0)
    b["poolM"] = M.reshape(128, 12 * 128)
    b["onesb"] = np.ones((128, 128))
    b["ident"] = np.eye(128)
    fo, o = {}, 0
    for kname, v in f.items():
        fo[kname] = (o, v.shape[1]); o += v.shape[1]
    bo, ob = {}, 0
    for kname, v in b.items():
        bo[kname] = (ob, v.shape[1]); ob += v.shape[1]
    cf = np.concatenate([f[kname] for kname in f], 1).astype(np.float32)
    cb = np.concatenate([b[kname] for kname in b], 1).astype(np.float32)
    return cf, fo, cb, bo


PCOL = {"nw": (0, 8), "adab": (8, 24), "pscale": (32, 4), "convw": (36, 32), "convb": (68, 8),
        "dskip": (76, 4), "ssmw": (80, 4), "fnw": (84, 8)}
NPCOL = 92
PROW = {"dtb": (0, 32), "alog": (32, 32), "sinks": (64, 8)}
NPROW = 72


def host_small(inp, l):
    pc = np.zeros((128, NPCOL), np.float32)
    pc[:, 0:8] = inp["norm_w"][l].reshape(8, 128).T
    pc[:, 8:32] = inp["ada_b"][l].reshape(24, 128).T
    pc[:, 32:36] = inp["pool_scale"][l].reshape(4, 128).T
    cw = inp["conv_w"][l]
    pc[:, 36:68] = cw.reshape(4, 8, 128).transpose(2, 1, 0).reshape(128, 32)
    pc[:, 68:76] = inp["conv_b"][l].reshape(8, 128).T
    ds = inp["d_skip"][l]
    pc[:, 76:80] = np.stack([np.where(np.arange(128) < 64, ds[2 * j], ds[2 * j + 1]) for j in range(4)], 1)
    pc[:, 80:84] = inp["ssm_norm_w"][l].reshape(4, 128).T
    pc[:, 84:92] = inp["final_norm_w"].reshape(8, 128).T
    pr = np.zeros((128, NPROW), np.float32)
    pr[:, 0:32] = np.tile(inp["dt_bias"][l], NCH)[None, :]
    pr[:, 32:64] = np.tile(inp["a_log"][l], NCH)[None, :]
    pr[:, 64:72] = inp["swa_sinks"][l][None, :]
    pw = np.ascontiguousarray(inp["pool_w"][l].transpose(1, 0, 2)).reshape(128, 512)
    return pc, pr, pw.astype(np.float32)


def build_program(NTOK, DEPTH):
    NT = NTOK // T
    woffs, WTOT = _weight_offsets()
    cf_np, FO, cb_np, BO = make_consts()
    NCF, NCB = cf_np.shape[1], cb_np.shape[1]
    nc = bass.Bass("TRN2", target_bir_lowering=False)
    xT_d = nc.dram_tensor("xT", [D, NTOK], F32, kind="ExternalInput").ap()
    outT_d = nc.dram_tensor("outT", [D, NTOK], F32, kind="ExternalOutput").ap()
    ccol_d = nc.dram_tensor("ccol", [128, 8], F32, kind="ExternalInput").ap()
    cstf_d = nc.dram_tensor("cstf", [128, NCF], F32, kind="ExternalInput").ap()
    cstb_d = nc.dram_tensor("cstb", [128, NCB], F32, kind="ExternalInput").ap()
    wts_d = [nc.dram_tensor("wts%d" % l, [128, WTOT], F32, kind="ExternalInput").ap() for l in range(DEPTH)]
    pcol_d = [nc.dram_tensor("pcol%d" % l, [128, NPCOL], F32, kind="ExternalInput").ap() for l in range(DEPTH)]
    prow_d = [nc.dram_tensor("prow%d" % l, [128, NPROW], F32, kind="ExternalInput").ap() for l in range(DEPTH)]
    poolw_d = [nc.dram_tensor("poolw%d" % l, [128, 512], F32, kind="ExternalInput").ap() for l in range(DEPTH)]
    xmid_d = [nc.dram_tensor("xmid%d" % l, [D, NTOK], F32).ap() for l in range(DEPTH - 1)]
    wbf_d = [nc.dram_tensor("wbf%d" % l, [128, WTOT], BF16).ap() for l in range(DEPTH)]
    S = Sched()
    dbg_outs = {}

    def dbg(name, ap, reads, dt=BF16):
        if not DBG:
            return
        shape = list(ap.shape)
        dd = nc.dram_tensor("dbg_" + name, shape, dt, kind="ExternalOutput").ap()
        dbg_outs[name] = dd
        S.op("sp", lambda e: e.dma_start(out=dd, in_=ap), reads=reads, dma="d_dbg_" + name)
        S.finals.append(("sp", [("d_dbg_" + name, 16)]))

    with ExitStack() as st:
        def sb(name, shape, dt):
            return st.enter_context(nc.sbuf_tensor(name, shape, dt))

        def psum(name, shape, dt):
            return st.enter_context(nc.psum_tensor(name, shape, dt))

        slots = [sb("slot%d" % i, [128, SLOTW], BF16) for i in range(NSLOT)]
        xTs = [sb("xTb%d" % i, [128, KC, T], F32) for i in range(2)]
        hT = sb("hT", [128, KC, T], BF16)
        acc = sb("acc", [128, KC, T], F32)
        QM = sb("QM", [128, KC, T], BF16)
        brs = [sb("br%d" % i, [128, 4, T], BF16) for i in range(2)]
        mgs = [sb("mgs%d" % i, [128, T], F32) for i in range(2)]
        sg = sb("sg", [128, 4, T], BF16)
        NF, NB = 7, 6
        f32s = [sb("f32s%d" % i, [128, 512], F32) for i in range(NF)]
        bfs = [sb("bfs%d" % i, [128, 512], BF16) for i in range(NB)]
        raws = [sb("raw%d" % i, [128, T + 4], BF16) for i in range(3)]
        dg = sb("dg", [128, 8, 4, 128], BF16)
        rnorm = sb("rnorm", [128, T], F32)
        qd = sb("qd", [128, 4, T], BF16)
        kT = sb("kT", [128, 4, T], BF16)
        vx = sb("vx", [128, NCH, 512], BF16)
        kdp = sb("kdp", [128, NCH, 4, 128], BF16)
        rstate = sb("rstate", [128, 256], F32)
        rstate_b = sb("rstate_b", [128, NV, 256], BF16)
        KTb = sb("KTb", [128, (NCH + 1) * 128], BF16)
        Vd = sb("Vd", [128, NCH + 1, 256], BF16)
        U = sb("U", [128, NCH + 1, 512], BF16)
        rawc = sb("rawc", [128, 8, 4], BF16)
        Btok = sb("Btok", [128, NCH, 256], BF16)
        xdp = sb("xdp", [128, NCH, 8, 128], BF16)
        acsT = sb("acsT", [8, T], F32)
        sm = sb("sm", [128, 8, 32], F32)
        sstate = sb("sstate", [128, 512], F32)
        sstate_b = sb("sstate_b", [128, NV, 512], BF16)
        cstf = sb("cstf_s", [128, NCF], F32)
        cstb = sb("cstb_s", [128, NCB], BF16)
        pcol = [sb("pcol_s%d" % l, [128, NPCOL], F32) for l in range(DEPTH)]
        prow = [sb("prow_s%d" % l, [128, NPROW], F32) for l in range(DEPTH)]
        poolw = [sb("poolw_s%d" % l, [128, 4, 128], BF16) for l in range(DEPTH)]
        modc = [sb("modc%d" % l, [128, 32], F32) for l in range(DEPTH)]
        sexp = [sb("sexp%d" % l, [128, 8], F32) for l in range(DEPTH)]
        ccol = sb("ccol_s", [128, 8], F32)
        scb = sb("scb", [128, 8], BF16)
        pbanks = [psum("pb%d" % i, [128, 512], F32) for i in range(7)]
        ptr = psum("ptr", [128, 1024], BF16)

        def CFv(name):
            o, n = FO[name]
            return cstf[:, o:o + n]

        def CBv(name):
            o, n = BO[name]
            return cstb[:, o:o + n]

        class Rot:
            def __init__(self, items, name):
                self.items, self.name, self.i = items, name, 0

            def next(self):
                k = self.i % len(self.items)
                self.i += 1
                return self.items[k], "%s%d" % (self.name, k)

        PS = Rot(pbanks, "ps")
        class RotPT:
            def __init__(self):
                self.i = 0

            def next(self):
                k = self.i % 2
                self.i += 1
                return ptr[:, k * 512:(k + 1) * 512], "ptr"
        PT = RotPT()
        FS = Rot(f32s, "fs")
        BS = Rot(bfs, "bs")
        RW = Rot(raws, "rw")
        MS = Rot(mgs, "ms")
        from collections import deque
        fillers = deque()

        def fill(k=1):
            for _ in range(k):
                if fillers:
                    ph = S.phase
                    fillers.popleft()[1]()
                    S.phase = ph

        def flush(upto=99):
            while fillers and fillers[0][0] <= upto:
                fill()

        def grp(fns):
            def f(e):
                last = None
                for fn in fns:
                    last = fn(e)
                return last
            f.nins = len(fns)
            return f

        def mm(out, lhsT, rhs, start=True, stop=True):
            return lambda e: e.matmul(out, lhsT=lhsT, rhs=rhs, start=start, stop=stop)

        def PE(fns, reads, writes):
            return S.op("pe", grp(fns), reads=reads, writes=writes)

        def ACT(out, in_, func, reads, writes, bias=None, scale=None):
            kw = {}
            if bias is not None:
                kw["bias"] = bias
            if scale is not None:
                kw["scale"] = scale
            return S.op("act", lambda e: e.activation(out=out, in_=in_, func=func, **kw), reads=reads, writes=writes)

        def TT(out, in0, in1, op, reads, writes, eng="dve"):
            return S.op(eng, lambda e: e.tensor_tensor(out=out, in0=in0, in1=in1, op=op), reads=reads, writes=writes)

        def TS(out, in0, s1, op0, reads, writes, s2=None, op1=None, eng="dve"):
            if op1 is None:
                return S.op(eng, lambda e: e.tensor_scalar(out=out, in0=in0, scalar1=s1, scalar2=0.0, op0=op0, op1=ALU.add), reads=reads, writes=writes)
            return S.op(eng, lambda e: e.tensor_scalar(out=out, in0=in0, scalar1=s1, scalar2=s2, op0=op0, op1=op1), reads=reads, writes=writes)

        def STT(out, in0, scalar, in1, op0, op1, reads, writes, eng="dve"):
            return S.op(eng, lambda e: e.scalar_tensor_tensor(out=out, in0=in0, scalar=scalar, in1=in1, op0=op0, op1=op1), reads=reads, writes=writes)

        def PRELOAD():
            return

        def RSQ(out, in_, eps_n, reads, writes):
            S.op("act", lambda e: e.activation(out=out, in_=in_, func=AF.Ln, bias=cst_eps[eps_n], scale=1.0 / eps_n), reads=reads, writes=writes)
            S.op("act", lambda e: e.activation(out=out, in_=out, func=AF.Exp, scale=-0.5), reads=writes, writes=writes)

        def CP(out, in_, reads, writes, eng="dve"):
            if eng == "act":
                return S.op("act", lambda e: e.copy(out=out, in_=in_), reads=reads, writes=writes)
            return S.op(eng, lambda e: e.tensor_copy(out=out, in_=in_), reads=reads, writes=writes)

        ring = {"n": 0}

        class Lazy:
            def __init__(self, l, name, first):
                self.a = (l, name, first); self.v = None

            def __getitem__(self, k):
                if self.v is None:
                    self.v = load_gran(*self.a)
                return self.v[k]

        def load_gran(l, name, first=True):
            o, n = woffs[name]
            k = ring["n"] % NSLOT
            ring["n"] += 1
            slot = slots[k]
            key = "slot%d" % k
            dkey = "wbf%d_%s" % (l, name)
            if first or name.startswith("ada"):
                src = wts_d[l][:, o:o + n]
                S.op("pool", lambda e: e.dma_start(out=slot[:, 0:n], in_=src), writes=[key], dma="d_ring%d" % k)
                if not name.startswith("ada"):
                    dst = wbf_d[l][:, o:o + n]
                    S.op("sp", lambda e: e.dma_start(out=dst, in_=slot[:, 0:n]), reads=[key], writes=[dkey], dma="d_wst%d" % k)
            else:
                src = wbf_d[l][:, o:o + n]
                S.op("sp", lambda e: e.dma_start(out=slot[:, 0:n], in_=src), reads=[dkey], writes=[key], dma="d_ring%d" % k)
            return slot, key, n

        S.op("sp", lambda e: e.dma_start(out=cstf[:], in_=cstf_d), writes=["cstf"], dma="d_c0")
        S.op("pool", lambda e: e.dma_start(out=cstb[:], in_=cstb_d), writes=["cstb"], dma="d_cb0")
        S.op("sp", lambda e: e.dma_start(out=ccol[:], in_=ccol_d), writes=["ccol"], dma="d_c1")
        for l in range(DEPTH):
            S.op("sp", (lambda l: lambda e: e.dma_start(out=pcol[l][:], in_=pcol_d[l]))(l), writes=["pcol%d" % l], dma="d_pc%d" % l)
            S.op("sp", (lambda l: lambda e: e.dma_start(out=prow[l][:], in_=prow_d[l]))(l), writes=["prow%d" % l], dma="d_pr%d" % l)
            S.op("pool", (lambda l: lambda e: e.dma_start(out=poolw[l][:].rearrange("p a b -> p (a b)"), in_=poolw_d[l]))(l),
                 writes=["poolw%d" % l], dma="d_pw%d" % l)
        ACT(scb[:], ccol[:], AF.Silu, ["ccol"], ["scb"])
        S.op("dve", lambda e: e.memset(kdp[:].rearrange("p a b c -> p (a b c)"), 0.0), writes=["kdp%d" % c for c in range(NCH)])
        S.op("dve", lambda e: e.memset(xdp[:].rearrange("p a b c -> p (a b c)"), 0.0), writes=["xdp%d" % c for c in range(NCH)])
        S.op("dve", lambda e: e.memset(rawc[:].rearrange("p a b -> p (a b)"), 0.0), writes=["rawc"])

        _eo = FO["epsc"][0]
        cst_eps = {D: cstf[:, _eo:_eo + 1], 128: cstf[:, _eo + 1:_eo + 2], 256: cstf[:, _eo + 2:_eo + 3]}
        onesb = CBv("onesb")
        ident = CBv("ident")
        tri = CFv("tri")
        onesf = CFv("ones")

        def layer_setup(l):
            pb, pk = PS.next()
            for gi in range(6):
                slot, key, n = load_gran(l, "ada%d" % gi)
                sv = slot[:, 0:4096].rearrange("p (k c) -> p k c", k=KC)
                fns = []
                for jj in range(4):
                    jt = gi * 4 + jj
                    for kc in range(KC):
                        fns.append(mm(pb[:, jt:jt + 1], sv[:, kc, jj * 128:(jj + 1) * 128], scb[:, kc:kc + 1], kc == 0, kc == KC - 1))
                PE(fns, [key, "scb"], [pk] if gi == 0 else [pk])
            o, n = PCOL["adab"]
            TT(modc[l][:, 0:24], pb[:, 0:24], pcol[l][:, o:o + n], ALU.add, [pk, "pcol%d" % l], ["modc%d" % l])
            o, n = PCOL["nw"]
            STT(modc[l][:, 24:32], modc[l][:, 8:16], 1.0, pcol[l][:, o:o + n], ALU.add, ALU.mult, ["modc%d" % l, "pcol%d" % l], ["modc%d" % l])
            o, n = PROW["sinks"]
            ACT(sexp[l][:], prow[l][:, o:o + n], AF.Exp, ["prow%d" % l], ["sexp%d" % l])
            o, n = PROW["alog"]
            ACT(prow[l][:, o:o + n], prow[l][:, o:o + n], AF.Exp, ["prow%d" % l], ["prow%d" % l])
            TS(prow[l][:, o:o + n], prow[l][:, o:o + n], -1.0, ALU.mult, ["prow%d" % l], ["prow%d" % l])

        for l in range(DEPTH):
            layer_setup(l)

        def proj_F(slot, key, c0, ntiles, evac):
            sv = slot[:, 0:4096].rearrange("p (k c) -> p k c", k=KC) if False else None
            for i in range(ntiles):
                pb, pk = PS.next()
                fns = []
                for kc in range(KC):
                    fns.append(mm(pb[:, 0:T], slotview(slot, kc, c0 + i * 128, 128), hT[:, kc, :], kc == 0, kc == KC - 1))
                PE(fns, [key, "hT"], [pk])
                evac(i, pb, pk)

        gw = {"w": 512}

        def slotview(slot, kc, c0, n):
            w = gw["w"]
            return slot[:, kc * w + c0: kc * w + c0 + n]

        def proj_T(slot, key, c0, ncols, evac):
            for c in range(NCH):
                pb, pk = PS.next()
                fns = []
                for kc in range(KC):
                    fns.append(mm(pb[:, 0:ncols], hT[:, kc, c * 128:(c + 1) * 128], slotview(slot, kc, c0, ncols), kc == 0, kc == KC - 1))
                PE(fns, [key, "hT"], [pk])
                evac(c, pb, pk)

        def merge(l, i, last, first, brt, brk):
            st_ = {}

            def step(jd):
                S.phase = "merge%d" % i
                hf = jd // 4
                if jd % 4 == 0:
                    st_["m"] = load_gran(l, "mg%d_%d" % (i, hf), first)
                    st_["u"] = load_gran(l, "up%d_%d" % (i, hf), first)
                mslot, mkey, _ = st_["m"]
                uslot, ukey, _ = st_["u"]
                pg_, pgk = PS.next()
                fns = [mm(pg_[:, 0:T], mslot[:, kc * 512 + (jd % 4) * 128: kc * 512 + (jd % 4 + 1) * 128], hT[:, kc, :], kc == 0, kc == KC - 1) for kc in range(KC)]
                PE(fns, [mkey, "hT"], [pgk])
                pu, puk = PS.next()
                fns = [mm(pu[:, 0:T], uslot[:, kc * 512 + (jd % 4) * 128: kc * 512 + (jd % 4 + 1) * 128], brt[:, kc, :], kc == 0, kc == 3) for kc in range(4)]
                PE(fns, [ukey, brk], [puk])
                sgm, sk_ = MS.next()
                ACT(sgm[:, 0:T], pg_[:, 0:T], AF.Exp, [pgk], [sk_], scale=-1.0)
                ACT(sgm[:, 0:T], sgm[:, 0:T], AF.Ln, [sk_], [sk_], bias=1.0)
                ACT(sgm[:, 0:T], sgm[:, 0:T], AF.Exp, [sk_], [sk_], scale=-1.0)
                if i == 0:
                    TT(acc[:, jd, :], pu[:, 0:T], sgm[:, 0:T], ALU.mult, [puk, sk_], ["acc%d" % jd])
                else:
                    TT(sgm[:, 0:T], pu[:, 0:T], sgm[:, 0:T], ALU.mult, [puk, sk_], [sk_])
                    if last:
                        TT(QM[:, jd, :], acc[:, jd, :], sgm[:, 0:T], ALU.add, ["acc%d" % jd, sk_], ["QM%d" % jd], eng="pool")
                    else:
                        TT(acc[:, jd, :], acc[:, jd, :], sgm[:, 0:T], ALU.add, ["acc%d" % jd, sk_], ["acc%d" % jd], eng="pool")
            for jd in range(KC):
                fillers.append((i, (lambda jd: lambda: step(jd))(jd)))

        def issue_x_load(gi):
            l, n = gi // NT, gi % NT
            if l >= DEPTH:
                return
            src = xT_d if l == 0 else xmid_d[l - 1]
            xb = xTs[gi % 2]
            xk = ["xT%d_%d" % (gi % 2, kc) for kc in range(KC)]
            srcv = src.rearrange("(k p) t -> p k t", p=128)[:, :, n * T:(n + 1) * T]
            S.op("pool", lambda e: e.dma_start(out=xb[:], in_=srcv), reads=["dram_x%d_%d" % (l, n)], writes=xk, dma="d_x%d" % (gi % 2))

        def tile_body(l, n):
            first = (n == 0)
            G0 = n * NCH
            gi = l * NT + n
            xT = xTs[gi % 2]
            dst = outT_d if l == DEPTH - 1 else xmid_d[l]
            xkeys = ["xT%d_%d" % (gi % 2, kc) for kc in range(KC)]
            dstv = dst.rearrange("(k p) t -> p k t", p=128)[:, :, n * T:(n + 1) * T]
            if n == 0:
                issue_x_load(gi)
            if (gi + 1) % NT != 0 or True:
                if (gi + 1) // NT == l:
                    issue_x_load(gi + 1)
            pc, pr = pcol[l], prow[l]
            pck, prk, mck = "pcol%d" % l, "prow%d" % l, "modc%d" % l

            S.phase = "norm"
            PRELOAD()
            pb, pk = PS.next()
            for kc in range(KC):
                sq, sqk = BS.next()
                ACT(sq[:, 0:T], xT[:, kc, :], AF.Square, [xkeys[kc]], [sqk])
                PE([mm(pb[:, 0:T], onesb, sq[:, 0:T], kc == 0, kc == KC - 1)], [sqk, "cstb"], [pk])
            r, rk = rnorm, "rnorm"
            if l == 0 and n == 0 and False:
                ss_, ssk = FS.next()
                CP(ss_[:, 0:T], pb[:, 0:T], [pk], [ssk])
                dbg("ssq", ss_[:, 0:T], [ssk], F32)
            RSQ(r[:, 0:T], pb[:, 0:T], D, [pk], [rk])
            pass
            for kc in range(KC):
                t_, tk = FS.next()
                STT(t_[:, 0:T], xT[:, kc, :], modc[l][:, 24 + kc:25 + kc], r[:, 0:T], ALU.mult, ALU.mult, [xkeys[kc], mck, rk], [tk])
                ACT(hT[:, kc, :], t_[:, 0:T], AF.Identity, [tk, mck], ["hT"], bias=modc[l][:, kc:kc + 1])

            S.phase = "A_proj"
            br, brk_ = brs[0], "br0"
            gw["w"] = 512
            sA_v = Lazy(l, "A_v", first)
            sA_qk = Lazy(l, "A_qk", first)
            sA_g = Lazy(l, "A_g", first)
            gw["w"] = 264
            sA_k = Lazy(l, "A_k", first)
            gw["w"] = 512
            qdm = CFv("qdm").rearrange("p (h t) -> p h t", h=4)

            def ev_qk(i, pb, pk):
                if i < 2:
                    for half in range(2):
                        h = 2 * i + half
                        TT(qd[:, h, :].rearrange("p (c t) -> p c t", c=NCH), pb[:, 0:T].rearrange("p (c t) -> p c t", c=NCH),
                           qdm[:, h:h + 1, :].to_broadcast([128, NCH, 128]), ALU.mult, [pk, "cstf"], ["qd%d" % h])
                else:
                    CP(kT[:, i - 2, :], pb[:, 0:T], [pk], ["kT%d" % (i - 2)], eng="act")
            proj_F(sA_qk[0], sA_qk[1], 0, 4, ev_qk)

            def ev_g(i, pb, pk):
                ACT(sg[:, i, :], pb[:, 0:T], AF.Silu, [pk], ["sg%d" % i])
            proj_F(sA_g[0], sA_g[1], 0, 4, ev_g)

            def ev_v(c, pb, pk):
                CP(vx[:, c, :], pb[:, 0:512], [pk], ["vx%d" % c], eng="act")
            proj_T(sA_v[0], sA_v[1], 0, 512, ev_v)
            kdec = CFv("kdec")

            def ev_k(c, pb, pk):
                pv = pb[:, 0:256].rearrange("p (j a d) -> p j a d", j=2, a=2)
                kv = kdp[:, c, :, :].rearrange("p (j a) x -> p j a x", a=2)
                kd4 = kdec.rearrange("p (j a) -> p j a", a=2)
                for a in range(2):
                    TT(kv[:, :, a, a * 64:(a + 1) * 64], pv[:, :, a, :], kd4[:, :, a:a + 1].to_broadcast([128, 2, 64]), ALU.mult,
                       [pk, "cstf"], ["kdp%d" % c])
            gw["w"] = 264
            proj_T(sA_k[0], sA_k[1], 0, 256, ev_k)
            S.phase = "D_dt"
            pb, pk = PS.next()
            fns = []
            for c in range(NCH):
                for kc in range(KC):
                    fns.append(mm(pb[:, c * 8:(c + 1) * 8], hT[:, kc, c * 128:(c + 1) * 128], slotview(sA_k[0], kc, 256, 8), kc == 0, kc == KC - 1))
            PE(fns, [sA_k[1], "hT"], [pk])
            o_db, _ = PROW["dtb"]
            o_al, _ = PROW["alog"]
            TT(sm[:, 6, :], pb[:, 0:32], pr[:, o_db:o_db + 32], ALU.add, [pk, prk], ["sm6"])
            ACT(sm[:, 6, :], sm[:, 6, :], AF.Exp, ["sm6"], ["sm6"])
            ACT(sm[:, 0, :], sm[:, 6, :], AF.Ln, ["sm6"], ["sm0"], bias=1.0)
            TT(sm[:, 1, :], sm[:, 0, :], pr[:, o_al:o_al + 32], ALU.mult, ["sm0", prk], ["sm1"])
            pb, pk = PS.next()
            PE([mm(pb[:, c * 8:(c + 1) * 8], tri, sm[:, 1, c * 8:(c + 1) * 8]) for c in range(NCH)] +
               [mm(pb[:, 32:64], onesf, sm[:, 1, :])], ["sm1", "cstf"], [pk])
            CP(sm[:, 2, :], pb[:, 0:32], [pk], ["sm2"])
            TT(sm[:, 6, :], pb[:, 32:64], sm[:, 2, :], ALU.subtract, [pk, "sm2"], ["sm6"])
            ACT(sm[:, 3, :], sm[:, 6, :], AF.Exp, ["sm6"], ["sm3"])
            ACT(sm[:, 4, :], pb[:, 32:64], AF.Exp, [pk], ["sm4"])
            TT(sm[:, 5, :], sm[:, 0, :], sm[:, 3, :], ALU.mult, ["sm0", "sm3"], ["sm5"])
            pb, pk = PS.next()
            PE([mm(pb[0:8, c * 128:(c + 1) * 128], sm[:, 1, c * 8:(c + 1) * 8], tri) for c in range(NCH)], ["sm1", "cstf"], [pk])
            CP(acsT[:, :], pb[0:8, 0:T], [pk], ["acsT"])
            gw["w"] = 512
            S.phase = "A_proj"
            S.phase = "A_state"
            gL = CFv("gL")
            for c in range(NCH):
                G = G0 + c
                pb, pk = PS.next()
                fns = []
                for j in range(2):
                    for a in range(2):
                        h = 2 * j + a
                        fns.append(mm(pb[:, j * 128:(j + 1) * 128], kdp[:, c, h, :], vx[:, c, h * 128:(h + 1) * 128], a == 0, a == 1))
                PE(fns, ["kdp%d" % c, "vx%d" % c], [pk])
                vnext = (G + 1) % NV
                if G == 0:
                    CP(rstate[:], pb[:, 0:256], [pk], ["rstate"])
                else:
                    for j in range(2):
                        STT(rstate[:, j * 128:(j + 1) * 128], rstate[:, j * 128:(j + 1) * 128], gL[:, j:j + 1], pb[:, j * 128:(j + 1) * 128],
                            ALU.mult, ALU.add, ["rstate", pk, "cstf"], ["rstate"])
                CP(rstate_b[:, vnext, :], rstate[:], ["rstate"], ["rsb%d" % vnext])
            maskTA = CFv("maskTA")
            sc_ps = {}

            def A_scores(c):
                pb, pk = PS.next()
                fns = []
                for h in range(4):
                    fns.append(mm(pb[:, h * 128:(h + 1) * 128], kT[:, h // 2, c * 128:(c + 1) * 128], qd[:, h, c * 128:(c + 1) * 128]))
                PE(fns, ["kT0", "kT1"] + ["qd%d" % h for h in range(4)], [pk])
                stt, stk = BS.next()
                TT(stt[:, :], pb[:, :], maskTA, ALU.mult, [pk, "cstf"], [stk])
                sc_ps[c] = (stt, stk)

            def A_out(c):
                G = G0 + c
                stt, stk = sc_ps[c]
                pb, pk = PS.next()
                fns = []
                ver = G % NV
                for h in range(4):
                    fns.append(mm(pb[:, h * 128:(h + 1) * 128], vx[:, c, h * 128:(h + 1) * 128], stt[:, h * 128:(h + 1) * 128], True, G == 0))
                    if G > 0:
                        fns.append(mm(pb[:, h * 128:(h + 1) * 128], rstate_b[:, ver, (h // 2) * 128:(h // 2 + 1) * 128], qd[:, h, c * 128:(c + 1) * 128], False, True))
                PE(fns, ["vx%d" % c, stk, "rsb%d" % ver] + ["qd%d" % h for h in range(4)], [pk])
                sq, sqk = BS.next()
                ACT(sq[:, :], pb[:, :], AF.Square, [pk], [sqk])
                p2, p2k = PS.next()
                PE([mm(p2[:, :], onesb, sq[:, :])], [sqk, "cstb"], [p2k])
                r, rk = FS.next()
                RSQ(r[:, :], p2[:, :], 128, [p2k], [rk])
                TT(r[:, :], pb[:, :], r[:, :], ALU.mult, [pk, rk], [rk])
                TT(br[:, :, c * 128:(c + 1) * 128], r[:, :].rearrange("p (h t) -> p h t", h=4),
                   sg[:, :, c * 128:(c + 1) * 128], ALU.mult, [rk] + ["sg%d" % i for i in range(4)], [brk_])
            S.phase = "A_core"
            PRELOAD()
            A_scores(0)
            for c in range(NCH):
                if c + 1 < NCH:
                    A_scores(c + 1)
                A_out(c)
            if l == 0: dbg("brA%d" % n, br[:], [brk_])
            if l == 0: dbg("hT%d" % n, hT[:], ["hT"])
            if l == 0 and n == 0: dbg("modc", modc[l][:], [mck], F32)
            merge(l, 0, False, first, br, brk_)

            S.phase = "B_proj"
            br, brk_ = brs[1], "br1"
            gw["w"] = 512
            sB_q = Lazy(l, "B_q", first)
            gw["w"] = 384
            sB_kv = Lazy(l, "B_kv", first)
            gw["w"] = 512
            sB_g = Lazy(l, "B_g", first)
            rm8 = CFv("rowmask8")

            def ev_q(i, pb, pk):
                for g in range(2):
                    TS(QM[:, g * 4 + i, :], pb[:, 0:T], rm8[:, g:g + 1], ALU.mult, [pk, "cstf"], ["QM%d" % (g * 4 + i)])
            proj_F(sB_q[0], sB_q[1], 0, 4, ev_q)
            gw["w"] = 384

            def ev_kt(i, pb, pk):
                CP(KTb[:, 128:128 + T], pb[:, 0:T], [pk], ["KTb"], eng="act")
            proj_F(sB_kv[0], sB_kv[1], 0, 1, ev_kt)

            def ev_vd(c, pb, pk):
                CP(Vd[:, c + 1, :], pb[:, 0:256], [pk], ["Vd%d" % (c + 1)], eng="act")
            proj_T(sB_kv[0], sB_kv[1], 128, 256, ev_vd)
            gw["w"] = 512
            proj_F(sB_g[0], sB_g[1], 0, 4, ev_g)
            S.phase = "B_core"
            swadec = CBv("swadec").rearrange("p (g k x) -> p g k x", g=2, k=2)
            b_state = {}

            def B_s1(c, g):
                G = G0 + c
                kbs = [1] if G == 0 else [0, 1]
                ets = []
                for kb in kbs:
                    pb, pk = PS.next()
                    fns = [mm(pb[:, jj * 128:(jj + 1) * 128], KTb[:, (c + kb) * 128:(c + kb + 1) * 128], QM[:, g * 4 + jj, c * 128:(c + 1) * 128])
                           for jj in range(4)]
                    PE(fns, ["KTb"] + ["QM%d" % (g * 4 + jj) for jj in range(4)], [pk])
                    ex, exk = FS.next()
                    ACT(ex[:, :], pb[:, :], AF.Exp, [pk], [exk])
                    et, etk = BS.next()
                    TT(et[:, :], ex[:, :], swadec[:, g, kb, :], ALU.mult, [exk, "cstb"], [etk], eng="pool")
                    ets.append((et, etk, kb))
                b_state[(c, g)] = ets

            def B_s2(c, g):
                ets = b_state.pop((c, g))
                pden, pdk = PS.next()
                PE([mm(pden[:, :], onesb, et[:, :], i == 0, i == len(ets) - 1) for i, (et, etk, kb) in enumerate(ets)],
                   [e_[1] for e_ in ets] + ["cstb"], [pdk])
                ppv, ppk = PS.next()
                PE([mm(ppv[:, :], Vd[:, c + kb, g * 128:(g + 1) * 128], et[:, :], i == 0, i == len(ets) - 1) for i, (et, etk, kb) in enumerate(ets)],
                   [e_[1] for e_ in ets] + ["Vd%d" % (c + kb) for (_, _, kb) in ets], [ppk])
                t1, t1k = FS.next()
                for jj in range(4):
                    ACT(t1[:, jj * 128:(jj + 1) * 128], pden[:, jj * 128:(jj + 1) * 128], AF.Ln, [pdk, "sexp%d" % l], [t1k],
                        bias=sexp[l][:, g * 4 + jj:g * 4 + jj + 1])
                ACT(t1[:, :], t1[:, :], AF.Exp, [t1k], [t1k], scale=-1.0)
                TT(t1[:, :], ppv[:, :], t1[:, :], ALU.mult, [ppk, t1k], [t1k])
                lo, hi = g * 64, (g + 1) * 64
                TT(br[lo:hi, :, c * 128:(c + 1) * 128], t1[lo:hi, :].rearrange("p (j q) -> p j q", j=4), sg[lo:hi, :, c * 128:(c + 1) * 128],
                   ALU.mult, [t1k] + ["sg%d" % i for i in range(4)], [brk_])
            its = [(c, g) for c in range(NCH) for g in range(2)]
            PRELOAD()
            B_s1(*its[0])
            for i_, it in enumerate(its):
                if i_ + 1 < len(its):
                    B_s1(*its[i_ + 1])
                B_s2(*it)
                fill(1)
            CP(KTb[:, 0:128], KTb[:, NCH * 128:(NCH + 1) * 128], ["KTb"], ["KTb"])
            CP(Vd[:, 0, :], Vd[:, NCH, :], ["Vd%d" % NCH], ["Vd0"])
            if l == 0: dbg("brB%d" % n, br[:], [brk_])
            merge(l, 1, False, first, br, brk_)

            S.phase = "C_proj"
            br, brk_ = brs[0], "br0"
            sC_u = Lazy(l, "C_u", first)
            sC_g = Lazy(l, "C_g", first)

            def ev_u(c, pb, pk):
                CP(U[:, c + 1, :], pb[:, 0:512], [pk], ["U%d" % (c + 1)], eng="act")
            proj_T(sC_u[0], sC_u[1], 0, 512, ev_u)
            proj_F(sC_g[0], sC_g[1], 0, 4, ev_g)
            flush(0)
            S.phase = "C_core"
            poolM = CBv("poolM").rearrange("p (k i t) -> p k i t", k=3, i=4)
            o_ps, _ = PCOL["pscale"]
            for c in range(NCH):
                G = G0 + c
                pb, pk = PS.next()
                fns = []
                for i in range(4):
                    if G == 0:
                        fns.append(mm(pb[:, i * 128:(i + 1) * 128], U[:, c + 1, i * 128:(i + 1) * 128], poolM[:, 2, i, :]))
                    else:
                        fns.append(mm(pb[:, i * 128:(i + 1) * 128], U[:, c, i * 128:(i + 1) * 128], poolM[:, 1, i, :], True, False))
                        fns.append(mm(pb[:, i * 128:(i + 1) * 128], U[:, c + 1, i * 128:(i + 1) * 128], poolM[:, 0, i, :], False, True))
                PE(fns, ["U%d" % c, "U%d" % (c + 1), "cstb"], [pk])
                dT_, dk_ = BS.next()
                CP(dT_[:, :], pb[:, :], [pk], [dk_], eng="act")
                p2, p2k = PS.next()
                PE([mm(p2[:, i * 128:(i + 1) * 128], poolw[l][:, i, :], dT_[:, i * 128:(i + 1) * 128]) for i in range(4)], [dk_, "poolw%d" % l], [p2k])
                for i in range(4):
                    STT(br[:, i, c * 128:(c + 1) * 128], p2[:, i * 128:(i + 1) * 128], pc[:, o_ps + i:o_ps + i + 1], sg[:, i, c * 128:(c + 1) * 128],
                        ALU.mult, ALU.mult, [p2k, pck, "sg%d" % i], [brk_])
                fill(2)
            CP(U[:, 0, :], U[:, NCH, :], ["U%d" % NCH], ["U0"])
            if l == 0: dbg("brC%d" % n, br[:], [brk_])
            merge(l, 2, False, first, br, brk_)

            S.phase = "D_proj"
            br, brk_ = brs[1], "br1"
            sD_x0 = Lazy(l, "D_x0", first)
            sD_x1 = Lazy(l, "D_x1", first)
            sD_z = Lazy(l, "D_z", first)
            o_cw, _ = PCOL["convw"]
            o_cb, _ = PCOL["convb"]

            if first:
                for i in range(8):
                    for k in range(4):
                        TS(dg[:, i, k, :], ident, pc[:, o_cw + i * 4 + k:o_cw + i * 4 + k + 1], ALU.mult, ["cstb", pck], ["dg"])
            pend = []

            def conv_finish(i, rw, rwk):
                pcv, pcvk = PS.next()
                PE([mm(pcv[:, 0:T], dg[:, i, k, :], rw[:, k:k + T], k == 0, k == 3) for k in range(4)], ["dg", rwk], [pcvk])
                if i < 4:
                    ACT(qd[:, i, :], pcv[:, 0:T], AF.Silu, [pcvk, pck], ["qd%d" % i], bias=pc[:, o_cb + i:o_cb + i + 1])
                else:
                    ACT(kT[:, i - 4, :], pcv[:, 0:T], AF.Silu, [pcvk, pck], ["kT%d" % (i - 4)], bias=pc[:, o_cb + i:o_cb + i + 1])

            def mk_ev_xbc(base):
                def ev(i0, pb, pk):
                    i = base + i0
                    rw, rwk = RW.next()
                    if first:
                        S.op("dve", lambda e: e.memset(rw[:, 0:3], 0.0), writes=[rwk])
                    else:
                        CP(rw[:, 0:3], rawc[:, i, 0:3], ["rawc"], [rwk])
                    CP(rw[:, 3:3 + T], pb[:, 0:T], [pk, rwk], [rwk], eng="act")
                    CP(rawc[:, i, 0:3], rw[:, T:T + 3], [rwk], ["rawc"])
                    if pend:
                        conv_finish(*pend.pop())
                    pend.append((i, rw, rwk))
                return ev
            proj_F(sD_x0[0], sD_x0[1], 0, 4, mk_ev_xbc(0))
            proj_F(sD_x1[0], sD_x1[1], 0, 4, mk_ev_xbc(4))
            conv_finish(*pend.pop())
            proj_F(sD_z[0], sD_z[1], 0, 4, ev_g)
            sel = CFv("sel").rearrange("p (h m) -> p h m", h=12)
            S.phase = "D_tr"
            for c in range(NCH):
                pt, ptk = PT.next()
                S.op("pe", grp([(lambda j, pt, c: lambda e: e.transpose(pt[:, j * 128:(j + 1) * 128], qd[:, j, c * 128:(c + 1) * 128], ident))(j, pt, c) for j in range(4)]),
                     reads=["qd%d" % j for j in range(4)] + ["cstb"], writes=[ptk])
                pv = pt[:, 0:512].rearrange("p (j a d) -> p j a d", j=4, a=2)
                xv = xdp[:, c, :, :].rearrange("p (j a) x -> p j a x", a=2)
                dtv = sm[:, 0, c * 8:(c + 1) * 8].rearrange("p (j a) -> p j a", a=2)
                for a in range(2):
                    TT(xv[:, :, a, a * 64:(a + 1) * 64], pv[:, :, a, :], dtv[:, :, a:a + 1].to_broadcast([128, 4, 64]), ALU.mult,
                       [ptk, "sm0"], ["xdp%d" % c])
                TT(vx[:, c, :].rearrange("p (h d) -> p h d", h=8), pt[:, 0:512].rearrange("p (h d) -> p h d", h=8),
                   sm[:, 5, c * 8:(c + 1) * 8].unsqueeze(2).to_broadcast([128, 8, 64]), ALU.mult, [ptk, "sm5"], ["vx%d" % c])
                pt2, pt2k = PT.next()
                S.op("pe", grp([(lambda g, pt2, c: lambda e: e.transpose(pt2[:, g * 128:(g + 1) * 128], kT[:, g, c * 128:(c + 1) * 128], ident))(g, pt2, c) for g in range(2)]),
                     reads=["kT0", "kT1", "cstb"], writes=[pt2k])
                CP(Btok[:, c, :], pt2[:, 0:256], [pt2k], ["Btok%d" % c])
            S.phase = "D_state"
            for c in range(NCH):
                G = G0 + c
                pb, pk = PS.next()
                PE([mm(pb[:, g * 256:(g + 1) * 256], Btok[:, c, g * 128:(g + 1) * 128], vx[:, c, g * 256:(g + 1) * 256]) for g in range(2)],
                   ["Btok%d" % c, "vx%d" % c], [pk])
                vnext = (G + 1) % NV
                if G == 0:
                    CP(sstate[:], pb[:, :], [pk], ["sstate"])
                else:
                    TT(sstate[:].rearrange("p (h d) -> p h d", h=8), sstate[:].rearrange("p (h d) -> p h d", h=8),
                       sm[:, 4, c * 8:(c + 1) * 8].unsqueeze(2).to_broadcast([128, 8, 64]), ALU.mult, ["sstate", "sm4"], ["sstate"])
                    TT(sstate[:], sstate[:], pb[:, :], ALU.add, ["sstate", pk], ["sstate"])
                CP(sstate_b[:, vnext, :], sstate[:], ["sstate"], ["ssb%d" % vnext])
            flush(1)
            S.phase = "D_core"
            o_dk, _ = PCOL["dskip"]
            o_sw, _ = PCOL["ssmw"]
            d_state = {}

            def D_s1(c):
                pcb, pcbk = PS.next()
                PE([mm(pcb[:, g * 128:(g + 1) * 128], kT[:, g, c * 128:(c + 1) * 128], kT[:, 2 + g, c * 128:(c + 1) * 128]) for g in range(2)],
                   ["kT%d" % i for i in range(4)], [pcbk])
                cbm, cbk = FS.next()
                TT(cbm[:, 0:256].rearrange("p (g t) -> p g t", g=2), pcb[:, 0:256].rearrange("p (g t) -> p g t", g=2),
                   tri.unsqueeze(1).to_broadcast([128, 2, 128]), ALU.mult, [pcbk, "cstf"], [cbk])
                gts = []
                for g in range(2):
                    sgt, sgk = FS.next()
                    pbc, pbck = PS.next()
                    PE([mm(pbc[:, hh * 128:(hh + 1) * 128], sel[0:8, g * 4 + hh, :], acsT[:, c * 128:(c + 1) * 128]) for hh in range(4)], ["acsT", "cstf"], [pbck])
                    for hh in range(4):
                        h = g * 4 + hh
                        TS(sgt[:, hh * 128:(hh + 1) * 128], pbc[:, hh * 128:(hh + 1) * 128], sm[:, 2, c * 8 + h:c * 8 + h + 1], ALU.subtract, [pbck, "sm2"], [sgk],
                           s2=0.0, op1=ALU.min)
                    ACT(sgt[:, :], sgt[:, :], AF.Exp, [sgk], [sgk])
                    gt, gtk = BS.next()
                    TT(gt[:, :].rearrange("p (h t) -> p h t", h=4), sgt[:, :].rearrange("p (h t) -> p h t", h=4),
                       cbm[:, g * 128:(g + 1) * 128].unsqueeze(1).to_broadcast([128, 4, 128]), ALU.mult, [sgk, cbk], [gtk], eng="pool")
                    gts.append((gt, gtk))
                d_state[c] = gts

            def D_s2(c):
                G = G0 + c
                ver = G % NV
                gts = d_state.pop(c)
                pd, pdk = PS.next()
                fns = []
                for j in range(4):
                    g = j // 2
                    for a_ in range(2):
                        h = 2 * j + a_
                        fns.append(mm(pd[:, j * 128:(j + 1) * 128], xdp[:, c, h, :], gts[g][0][:, (h % 4) * 128:(h % 4 + 1) * 128], a_ == 0, a_ == 1))
                PE(fns, ["xdp%d" % c, gts[0][1], gts[1][1]], [pdk])
                y, yk = FS.next()
                if G > 0:
                    po, pok = PS.next()
                    PE([mm(po[:, j * 128:(j + 1) * 128], sstate_b[:, ver, j * 128:(j + 1) * 128], kT[:, 2 + j // 2, c * 128:(c + 1) * 128]) for j in range(4)],
                       ["ssb%d" % ver, "kT2", "kT3"], [pok])
                    pe_, pek = PS.next()
                    PE([mm(pe_[:, j * 128:(j + 1) * 128], sel[0:8, 8 + j, :], acsT[:, c * 128:(c + 1) * 128]) for j in range(4)], ["acsT", "cstf"], [pek])
                    ep, epk = FS.next()
                    ACT(ep[:, :], pe_[:, :], AF.Exp, [pek], [epk])
                    TT(ep[:, :], po[:, :], ep[:, :], ALU.mult, [pok, epk], [epk])
                    TT(y[:, :], pd[:, :], ep[:, :], ALU.add, [pdk, epk], [yk])
                else:
                    CP(y[:, :], pd[:, :], [pdk], [yk])
                for j in range(4):
                    STT(y[:, j * 128:(j + 1) * 128], qd[:, j, c * 128:(c + 1) * 128], pc[:, o_dk + j:o_dk + j + 1], y[:, j * 128:(j + 1) * 128],
                        ALU.mult, ALU.add, ["qd%d" % j, pck, yk], [yk])
                TT(y[:, :].rearrange("p (j t) -> p j t", j=4), y[:, :].rearrange("p (j t) -> p j t", j=4), sg[:, :, c * 128:(c + 1) * 128],
                   ALU.mult, [yk] + ["sg%d" % i for i in range(4)], [yk], eng="pool")
                sq, sqk = BS.next()
                ACT(sq[:, :], y[:, :], AF.Square, [yk], [sqk])
                p2, p2k = PS.next()
                PE([mm(p2[:, g * 128:(g + 1) * 128], onesb, sq[:, (2 * g + a_) * 128:(2 * g + a_ + 1) * 128], a_ == 0, a_ == 1) for g in range(2) for a_ in range(2)],
                   [sqk, "cstb"], [p2k])
                r, rk = FS.next()
                RSQ(r[:, 0:256], p2[:, 0:256], 256, [p2k], [rk])
                for j in range(4):
                    g = j // 2
                    STT(br[:, j, c * 128:(c + 1) * 128], y[:, j * 128:(j + 1) * 128], pc[:, o_sw + j:o_sw + j + 1], r[:, g * 128:(g + 1) * 128],
                        ALU.mult, ALU.mult, [yk, pck, rk], [brk_])
            PRELOAD()
            D_s1(0)
            for c in range(NCH):
                if c + 1 < NCH:
                    D_s1(c + 1)
                fill(1)
                D_s2(c)
                fill(1)
            if l == 0: dbg("brD%d" % n, br[:], [brk_])
            merge(l, 3, True, first, br, brk_)
            if l == 0: dbg("mrg%d" % n, QM[:], ["QM%d" % k_ for k_ in range(KC)])

            flush()
            S.phase = "outproj"
            wo = [Lazy(l, "wo_0", first), Lazy(l, "wo_1", first)]
            lastl = (l == DEPTH - 1)
            for jd in range(KC):
                wslot, wkey = wo[jd // 4][0], wo[jd // 4][1]
                pb, pk = PS.next()
                PE([mm(pb[:, 0:T], slotview(wslot, kc, (jd % 4) * 128, 128), QM[:, kc, :], kc == 0, kc == KC - 1) for kc in range(KC)],
                   [wkey] + ["QM%d" % kc for kc in range(KC)], [pk])
                STT(xT[:, jd, :], pb[:, 0:T], modc[l][:, 16 + jd:17 + jd], xT[:, jd, :], ALU.mult, ALU.add, [pk, mck, xkeys[jd]], [xkeys[jd]])
            if lastl:
                PRELOAD()
                pb, pk = PS.next()
                for kc in range(KC):
                    sq, sqk = BS.next()
                    ACT(sq[:, 0:T], xT[:, kc, :], AF.Square, [xkeys[kc]], [sqk])
                    PE([mm(pb[:, 0:T], onesb, sq[:, 0:T], kc == 0, kc == KC - 1)], [sqk, "cstb"], [pk])
                r, rk = FS.next()
                RSQ(r[:, 0:T], pb[:, 0:T], D, [pk], [rk])
                o_fn, _ = PCOL["fnw"]
                for kc in range(KC):
                    STT(xT[:, kc, :], xT[:, kc, :], pc[:, o_fn + kc:o_fn + kc + 1], r[:, 0:T], ALU.mult, ALU.mult, [xkeys[kc], pck, rk], [xkeys[kc]])
            S.op("pool", lambda e: e.dma_start(out=dstv, in_=xT[:]), reads=xkeys,
                 writes=["dram_x%d_%d" % (l + 1, n)], dma="d_o%d" % (gi % 2))

        for l in range(DEPTH):
            for n in range(NT):
                tile_body(l, n)
        S.finals.append(("pool", [("d_o0", S.cnt["d_o0"]), ("d_o1", S.cnt["d_o1"])]))
        print("sbuf bytes remaining", nc.sbuf_bytes_remaining)
        S.emit(nc)
    global LAST_SCHED
    LAST_SCHED = S
    return nc


_PROG = {}
DBG = False


def kernel(**inputs):
    inp = {k: np.asarray(v) for k, v in inputs.items()}
    x = inp["x"]
    B, NTOK, _ = x.shape
    DEPTH = inp["w_in"].shape[0]
    key = (NTOK, DEPTH)
    if key not in _PROG:
        _PROG[key] = build_program(NTOK, DEPTH)
    nc = _PROG[key]
    cf, _, cb, _ = make_consts()
    shared = {"cstf": cf, "cstb": cb}
    for l in range(DEPTH):
        w, _ = host_weights(inp, l)
        pc, pr, pw = host_small(inp, l)
        shared["wts%d" % l] = w
        shared["pcol%d" % l] = pc
        shared["prow%d" % l] = pr
        shared["poolw%d" % l] = pw
    in_maps = []
    for b in range(B):
        m = dict(shared)
        m["xT"] = np.ascontiguousarray(x[b].T)
        m["ccol"] = np.ascontiguousarray(inp["c"][b].reshape(8, 128).T)
        in_maps.append(m)
    res = run_bass_kernel_spmd(nc, in_maps, core_ids=list(range(B)))
    if DBG:
        global DBG_RES
        DBG_RES = res.results
    out = np.stack([np.ascontiguousarray(r["outT"].T) for r in res.results], 0)
    return out.astype(np.float32)
```
